# Optimizing a Trainium2 kernel written in Bass

```python
import math
import jax, jax.numpy as jnp
from jax import lax
import numpy as np

D_MODEL = 1024
BATCH = 8
SEQ = 2048
DEPTH = 4

CHUNK = 64
N_MIXERS = 4
ALPHA = (2.0 * DEPTH) ** 0.25
BETA = (8.0 * DEPTH) ** -0.25
LN_EPS = 1e-5

CONV_A_WIDTH = 3

RWKV_HEAD = 64
RWKV_HEADS = D_MODEL // RWKV_HEAD
RWKV_DECAY_LORA = 64
RWKV_AAA_LORA = 64
RWKV_GATE_LORA = 160
RWKV_GN_EPS = 64e-5
RWKV_N_MIX = 6

D_RNN = 1280
LRU_BLOCKS = 10
LRU_BW = D_RNN // LRU_BLOCKS
CONV_C_WIDTH = 4
LRU_C = 8.0

RET_HEADS = 4
RET_DK = D_MODEL // RET_HEADS
RET_DV = 2 * D_MODEL // RET_HEADS
ROPE_BASE = 10000.0
RET_GN_EPS = 1e-6

D_FF = 2816
N_EXPERTS = 8
TOP_K = 2
D_FF_EXPERT = 3584
N_DENSE = (DEPTH + 1) // 2
N_MOE = DEPTH // 2

kernel_name = "hybrid_streaming_encoder_trunk"


def layer_norm(x, g, b, eps=LN_EPS):
    xf = x.astype(jnp.float32)
    mu = jnp.mean(xf, -1, keepdims=True)
    var = jnp.mean(jnp.square(xf - mu), -1, keepdims=True)
    return ((xf - mu) * lax.rsqrt(var + eps) * g + b).astype(x.dtype)


def group_norm_heads(y, g, b, eps):
    yf = y.astype(jnp.float32)
    mu = jnp.mean(yf, -1, keepdims=True)
    var = jnp.mean(jnp.square(yf - mu), -1, keepdims=True)
    return (yf - mu) * lax.rsqrt(var + eps) * g + b


def head_rms_norm(y, eps):
    yf = y.astype(jnp.float32)
    return yf * lax.rsqrt(jnp.mean(jnp.square(yf), -1, keepdims=True) + eps)


def causal_dwconv(x, w):
    K = w.shape[0]
    S = x.shape[1]
    xp = jnp.pad(x, ((0, 0), (K - 1, 0), (0, 0)))
    return sum(xp[:, k:k + S] * w[k] for k in range(K))


def token_shift(x):
    return jnp.pad(x, ((0, 0), (1, 0), (0, 0)))[:, :-1]


def rotary(t, positions):
    half = t.shape[-1] // 2
    freq = ROPE_BASE ** -jnp.linspace(0.0, 1.0, half, dtype=jnp.float32)
    ang = positions.astype(jnp.float32)[..., None] * freq
    cos = jnp.cos(ang)[:, :, None, :]
    sin = jnp.sin(ang)[:, :, None, :]
    tf = t.astype(jnp.float32)
    t1, t2 = tf[..., :half], tf[..., half:]
    return jnp.concatenate([t1 * cos - t2 * sin, t1 * sin + t2 * cos], axis=-1)


def linear_recurrence(a, b):
    def combine(left, right):
        a_l, b_l = left
        a_r, b_r = right
        return a_l * a_r, a_r * b_l + b_r
    _, h = lax.associative_scan(combine, (a, b), axis=1)
    return h


def short_conv_mixer(x, w_in, conv_w, w_out):
    b_gate, c_gate, v = jnp.split(x @ w_in, 3, axis=-1)
    y = b_gate * causal_dwconv(c_gate * v, conv_w)
    return y @ w_out


def rwkv7_scan(r, w, k, v, a, b):
    Bsz, S, H, N = r.shape

    def step(state, inp):
        r_t, w_t, k_t, v_t, a_t, b_t = inp
        sa = jnp.einsum('bhvk,bhk->bhv', state, a_t)
        state = (state * w_t[:, :, None, :]
                 + sa[..., None] * b_t[:, :, None, :]
                 + v_t[..., None] * k_t[:, :, None, :])
        return state, jnp.einsum('bhvk,bhk->bhv', state, r_t)

    xs = tuple(jnp.moveaxis(t, 1, 0) for t in (r, w, k, v, a, b))
    s0 = jnp.zeros((Bsz, H, N, N), jnp.float32)
    _, y = lax.scan(step, s0, xs)
    return jnp.moveaxis(y, 0, 1)


def rwkv7_mixer(x, mix, w_r, w_k, w_v, w0, w1, w2, a0, a1, a2, g1, g2,
                k_k, k_a, r_k, gn_g, gn_b, w_o):
    Bsz, S, D = x.shape
    H, N = RWKV_HEADS, RWKV_HEAD
    xx = token_shift(x) - x
    xr, xw, xk, xv, xa, xg = (x + xx * mix[j] for j in range(RWKV_N_MIX))

    r = xr @ w_r
    w_log = -jax.nn.softplus(-(w0 + jnp.tanh(xw @ w1) @ w2)) - 0.5
    k = xk @ w_k
    v = xv @ w_v
    a = jax.nn.sigmoid(a0 + (xa @ a1) @ a2)
    g = jax.nn.sigmoid(xg @ g1) @ g2

    heads = lambda t: t.astype(jnp.float32).reshape(Bsz, S, H, N)
    kk = heads(k * k_k)
    kk = kk / jnp.maximum(jnp.sqrt(jnp.sum(jnp.square(kk), -1, keepdims=True)), 1e-12)
    k = k * (1 + (a - 1) * k_a)

    rh, kh, vh, ah = heads(r), heads(k), heads(v), heads(a)
    decay = jnp.exp(-jnp.exp(heads(w_log)))
    y = rwkv7_scan(rh, decay, kh, vh, -kk, kk * ah)

    y = group_norm_heads(y, gn_g.reshape(H, N), gn_b.reshape(H, N), RWKV_GN_EPS)
    bonus = jnp.sum(rh * kh * r_k, -1, keepdims=True) * vh
    y = (y + bonus).reshape(Bsz, S, D).astype(x.dtype)
    return (y * g) @ w_o


def rglru_mixer(x, positions, w_in, conv_w, conv_b, w_ga, b_ga, w_gx, b_gx, lam, w_out):
    Bsz, S, _ = x.shape
    gate, u = jnp.split(x @ w_in, 2, axis=-1)
    u = (causal_dwconv(u, conv_w) + conv_b).astype(jnp.float32)
    ub = u.reshape(Bsz, S, LRU_BLOCKS, LRU_BW)
    r = jax.nn.sigmoid(jnp.einsum('bsgi,gij->bsgj', ub, w_ga).reshape(Bsz, S, D_RNN) + b_ga)
    i = jax.nn.sigmoid(jnp.einsum('bsgi,gij->bsgj', ub, w_gx).reshape(Bsz, S, D_RNN) + b_gx)
    log_a = -LRU_C * r * jax.nn.softplus(-lam)
    reset = (positions == 0)[..., None]
    a = jnp.where(reset, 0.0, jnp.exp(log_a))
    mult = jnp.where(reset, 1.0, jnp.sqrt(-jnp.expm1(2.0 * log_a)))
    h = linear_recurrence(a, mult * (i * u))
    y = jax.nn.gelu(gate) * h.astype(x.dtype)
    return y @ w_out


def retention_mixer(x, positions, w_in, w_o):
    Bsz, S, D = x.shape
    H, DK, DV = RET_HEADS, RET_DK, RET_DV
    NC = S // CHUNK
    q, k, v, g = jnp.split(x @ w_in, [D, 2 * D, 4 * D], axis=-1)
    q = rotary(q.reshape(Bsz, S, H, DK), positions)
    k = rotary(k.reshape(Bsz, S, H, DK), positions) * (DK ** -0.5)
    v = v.astype(jnp.float32).reshape(Bsz, S, H, DV)

    def to_chunks(t):
        return t.reshape(Bsz, NC, CHUNK, H, t.shape[-1]).transpose(1, 0, 3, 2, 4)

    log_g = jnp.log1p(-(2.0 ** (-5.0 - jnp.arange(H, dtype=jnp.float32))))
    idx = jnp.arange(CHUNK, dtype=jnp.float32)
    intra = jnp.exp(jnp.abs(idx[:, None] - idx[None, :]) * log_g[:, None, None])
    q_dec = jnp.exp((idx + 1.0) * log_g[:, None])[..., None]
    k_dec = jnp.exp((CHUNK - 1.0 - idx) * log_g[:, None])[..., None]
    c_dec = jnp.exp(CHUNK * log_g)[:, None, None]

    def step(state, chunk):
        qc, kc, vc = chunk
        scores = jnp.einsum('bhid,bhjd->bhij', qc, kc) * intra
        out = (jnp.einsum('bhij,bhjv->bhiv', scores, vc)
               + jnp.einsum('bhid,bhdv->bhiv', qc * q_dec, state))
        state = state * c_dec + jnp.einsum('bhjd,bhjv->bhdv', kc * k_dec, vc)
        return state, out

    s0 = jnp.zeros((Bsz, H, DK, DV), jnp.float32)
    _, y = lax.scan(step, s0, (to_chunks(q), to_chunks(k), to_chunks(v)))
    y = y.transpose(1, 0, 3, 2, 4).reshape(Bsz, S, H, DV)
    y = head_rms_norm(y, RET_GN_EPS).reshape(Bsz, S, 2 * D).astype(x.dtype)
    return (jax.nn.silu(g) * y) @ w_o


def swiglu(x, w_gu, w_down):
    gt, up = jnp.split(x @ w_gu, 2, axis=-1)
    return (jax.nn.silu(gt) * up) @ w_down


def moe_ffn(x, w_router, w_gu, w_down):
    Bsz, S, D = x.shape
    xt = x.reshape(-1, D)
    logits = (xt @ w_router).astype(jnp.float32)
    top_v, top_i = lax.top_k(logits, TOP_K)
    probs = jax.nn.softmax(top_v, axis=-1)
    gates = jnp.sum(jax.nn.one_hot(top_i, N_EXPERTS, dtype=jnp.float32) * probs[..., None], axis=1)
    gates = gates.astype(x.dtype)
    out = jnp.zeros_like(xt)
    for e in range(N_EXPERTS):
        out = out + gates[:, e:e + 1] * swiglu(xt, w_gu[e], w_down[e])
    return out.reshape(Bsz, S, D)


def setup_inputs(seed: int = 0) -> dict:
    key = jax.random.key(seed)
    keys = jax.random.split(key, 64)
    counter = [0]

    def nxt():
        k = keys[counter[0]]
        counter[0] += 1
        return k

    def nrm(shape, scale):
        return jax.random.normal(nxt(), shape, jnp.float32) * scale

    def unif(shape, lo, hi):
        return jax.random.uniform(nxt(), shape, jnp.float32, lo, hi)

    D = D_MODEL
    inp = {}
    inp["x"] = nrm((BATCH, SEQ, D), 1.0)
    offsets = jax.random.randint(nxt(), (BATCH, 1), 0, 2, dtype=jnp.int32) * SEQ
    inp["positions"] = offsets + jnp.arange(SEQ, dtype=jnp.int32)[None, :]
    inp["ln_mix_g"] = 1.0 + nrm((DEPTH, D), 0.02)
    inp["ln_mix_b"] = nrm((DEPTH, D), 0.02)
    inp["ln_ffn_g"] = 1.0 + nrm((DEPTH, D), 0.02)
    inp["ln_ffn_b"] = nrm((DEPTH, D), 0.02)
    inp["a_w_in"] = nrm((D, 3 * D), D ** -0.5)
    inp["a_conv_w"] = nrm((CONV_A_WIDTH, D), 0.5)
    inp["a_w_out"] = nrm((D, D), BETA * D ** -0.5)
    inp["b_mix"] = unif((RWKV_N_MIX, D), 0.0, 1.0)
    inp["b_w_r"] = nrm((D, D), D ** -0.5)
    inp["b_w_k"] = nrm((D, D), D ** -0.5)
    inp["b_w_v"] = nrm((D, D), D ** -0.5)
    inp["b_w0"] = unif((D,), -6.0, -1.0)
    inp["b_w1"] = nrm((D, RWKV_DECAY_LORA), D ** -0.5)
    inp["b_w2"] = nrm((RWKV_DECAY_LORA, D), 0.5 * RWKV_DECAY_LORA ** -0.5)
    inp["b_a0"] = nrm((D,), 0.5)
    inp["b_a1"] = nrm((D, RWKV_AAA_LORA), D ** -0.5)
    inp["b_a2"] = nrm((RWKV_AAA_LORA, D), RWKV_AAA_LORA ** -0.5)
    inp["b_g1"] = nrm((D, RWKV_GATE_LORA), D ** -0.5)
    inp["b_g2"] = nrm((RWKV_GATE_LORA, D), RWKV_GATE_LORA ** -0.5)
    inp["b_k_k"] = 0.85 + nrm((D,), 0.05)
    inp["b_k_a"] = 1.0 + nrm((D,), 0.05)
    inp["b_r_k"] = nrm((RWKV_HEADS, RWKV_HEAD), 0.1)
    inp["b_gn_g"] = 1.0 + nrm((D,), 0.02)
    inp["b_gn_b"] = nrm((D,), 0.02)
    inp["b_w_o"] = nrm((D, D), BETA * D ** -0.5)
    inp["c_w_in"] = nrm((D, 2 * D_RNN), D ** -0.5)
    inp["c_conv_w"] = nrm((CONV_C_WIDTH, D_RNN), 0.5)
    inp["c_conv_b"] = nrm((D_RNN,), 0.02)
    inp["c_w_ga"] = nrm((LRU_BLOCKS, LRU_BW, LRU_BW), LRU_BW ** -0.5)
    inp["c_b_ga"] = nrm((D_RNN,), 0.02)
    inp["c_w_gx"] = nrm((LRU_BLOCKS, LRU_BW, LRU_BW), LRU_BW ** -0.5)
    inp["c_b_gx"] = nrm((D_RNN,), 0.02)
    a_pow = unif((D_RNN,), 0.9, 0.999) ** (1.0 / LRU_C)
    inp["c_lam"] = jnp.log(a_pow) - jnp.log1p(-a_pow)
    inp["c_w_out"] = nrm((D_RNN, D), BETA * D_RNN ** -0.5)
    inp["d_w_in"] = nrm((D, 6 * D), D ** -0.5)
    inp["d_w_o"] = nrm((2 * D, D), BETA * (2 * D) ** -0.5)
    inp["ffn_w_gu"] = nrm((N_DENSE, D, 2 * D_FF), D ** -0.5)
    inp["ffn_w_down"] = nrm((N_DENSE, D_FF, D), BETA * D_FF ** -0.5)
    inp["moe_w_router"] = nrm((N_MOE, D, N_EXPERTS), D ** -0.5)
    inp["moe_w_gu"] = nrm((N_MOE, N_EXPERTS, D, 2 * D_FF_EXPERT), D ** -0.5)
    inp["moe_w_down"] = nrm((N_MOE, N_EXPERTS, D_FF_EXPERT, D), BETA * D_FF_EXPERT ** -0.5)
    return inp


def reference(x, positions, ln_mix_g, ln_mix_b, ln_ffn_g, ln_ffn_b,
              a_w_in, a_conv_w, a_w_out,
              b_mix, b_w_r, b_w_k, b_w_v, b_w0, b_w1, b_w2, b_a0, b_a1, b_a2,
              b_g1, b_g2, b_k_k, b_k_a, b_r_k, b_gn_g, b_gn_b, b_w_o,
              c_w_in, c_conv_w, c_conv_b, c_w_ga, c_b_ga, c_w_gx, c_b_gx, c_lam, c_w_out,
              d_w_in, d_w_o,
              ffn_w_gu, ffn_w_down, moe_w_router, moe_w_gu, moe_w_down):
    mixers = (
        lambda h: short_conv_mixer(h, a_w_in, a_conv_w, a_w_out),
        lambda h: rwkv7_mixer(h, b_mix, b_w_r, b_w_k, b_w_v, b_w0, b_w1, b_w2, b_a0, b_a1, b_a2,
                              b_g1, b_g2, b_k_k, b_k_a, b_r_k, b_gn_g, b_gn_b, b_w_o),
        lambda h: rglru_mixer(h, positions, c_w_in, c_conv_w, c_conv_b, c_w_ga, c_b_ga,
                              c_w_gx, c_b_gx, c_lam, c_w_out),
        lambda h: retention_mixer(h, positions, d_w_in, d_w_o),
    )
    for i in range(DEPTH):
        x = layer_norm(ALPHA * x + mixers[i % N_MIXERS](x), ln_mix_g[i], ln_mix_b[i])
        j = i // 2
        if i % 2 == 0:
            f = swiglu(x, ffn_w_gu[j], ffn_w_down[j])
        else:
            f = moe_ffn(x, moe_w_router[j], moe_w_gu[j], moe_w_down[j])
        x = layer_norm(ALPHA * x + f, ln_ffn_g[i], ln_ffn_b[i])
    return x
```

```python
import math
import numpy as np
from contextlib import ExitStack
import concourse.bass as bass
import concourse.mybir as mybir
from concourse.bass_utils import run_bass_kernel_spmd

F32 = mybir.dt.float32
BF16 = mybir.dt.bfloat16
I32 = mybir.dt.int32
ALU = mybir.AluOpType
AF = mybir.ActivationFunctionType
AX = mybir.AxisListType

D = 1024
S = 2048
KC = 8
TT = 512
NT = S // TT
DEPTH = 4
ALPHA = (2.0 * DEPTH) ** 0.25
LN_EPS = 1e-5
D_FF = 2816
D_FFE = 3584
NE = 8
D_RNN = 1280
SEM_M = 30000
N_CORES = 8


class Buf:
    __slots__ = ("name", "lw", "rd")

    def __init__(self, name):
        self.name = name
        self.lw = None
        self.rd = {}


class View:
    __slots__ = ("bufs", "ap")

    def __init__(self, bufs, ap):
        self.bufs = tuple(bufs)
        self.ap = ap

    def __getitem__(self, idx):
        return View(self.bufs, self.ap[idx])

    def re(self, pattern_, **kw):
        return View(self.bufs, self.ap.rearrange(pattern_, **kw))

    def bc(self, shape):
        return View(self.bufs, self.ap.to_broadcast(shape))

    def cast(self, dt):
        return View(self.bufs, self.ap.bitcast(dt))


class Tile:
    def __init__(self, name, t):
        self.buf = Buf(name)
        self.t = t

    def __getitem__(self, idx):
        return View((self.buf,), self.t[idx])

    def v(self):
        return View((self.buf,), self.t[:])


class Grid:
    def __init__(self, name, t, C, T, tile):
        self.t = t
        self.C, self.T, self.tile = C, T, tile
        self.bufs = [[Buf(f"{name}_{c}_{i}") for i in range((T + tile - 1) // tile)] for c in range(C)]

    def v(self, c, t0, t1):
        i0, i1 = t0 // self.tile, (t1 - 1) // self.tile
        return View(self.bufs[c][i0:i1 + 1], self.t[:, c, t0:t1])

    def tl(self, c, i):
        return self.v(c, i * self.tile, min(self.T, (i + 1) * self.tile))

    def full(self, c):
        return self.v(c, 0, self.T)

    def multi(self, c0, c1, t0, t1):
        i0, i1 = t0 // self.tile, (t1 - 1) // self.tile
        bufs = [b for c in range(c0, c1) for b in self.bufs[c][i0:i1 + 1]]
        return View(bufs, self.t[:, c0:c1, t0:t1])


ENGS = ("pe", "act", "dve", "pool", "sp")


class Prog:
    def __init__(self, nc, stack):
        self.nc = nc
        self.st = stack
        self.ops = {e: [] for e in ENGS}
        self.cnt = {e: 0 for e in ENGS}
        self.seen = {e: {} for e in ENGS}
        self.sems = {}
        self.same_sync = {"pe": False, "act": True, "dve": True, "pool": True, "sp": False}
        self.dma_pool = {}
        self.dma_rr = {}
        self.ndma = {"sp": 12, "pool": 12, "act": 8}
        self.uid = 0
        self.ps_rr = 0
        self.out_tokens = []

    def name(self, p):
        self.uid += 1
        return f"{p}_{self.uid}"

    def sem(self, key):
        if key not in self.sems:
            self.sems[key] = self.st.enter_context(self.nc.semaphore(self.name("s_" + "_".join(str(k) for k in key))))
        return self.sems[key]

    def sb(self, shape, dt, name="t", st=None):
        st = st or self.st
        nm = self.name(name)
        t = st.enter_context(self.nc.sbuf_tensor(nm, list(shape), dt))
        return Tile(nm, t)

    def grid(self, C, T, dt, name="g", tile=TT, st=None):
        st = st or self.st
        nm = self.name(name)
        t = st.enter_context(self.nc.sbuf_tensor(nm, [128, C, T], dt))
        return Grid(nm, t, C, T, tile)

    def pst(self, shape, dt, name="ps"):
        nm = self.name(name)
        t = self.st.enter_context(self.nc.psum_tensor(nm, list(shape), dt))
        return Tile(nm, t)

    def _tok_engine(self, eng):
        idx = self.cnt[eng]
        self.cnt[eng] += 1
        key = (eng, idx // SEM_M)
        self.sem(key)
        return (key, idx % SEM_M + 1)

    def _tok_dma(self, eng, waits):
        if eng not in self.dma_pool:
            self.dma_pool[eng] = [[("dma", eng, i), 0] for i in range(self.ndma[eng])]
            self.dma_rr[eng] = 0
        pool = self.dma_pool[eng]
        ent = pool[self.dma_rr[eng] % len(pool)]
        self.dma_rr[eng] += 1
        key, last = ent
        self.sem(key)
        if last > 0 and self.seen[eng].get(key, 0) < last:
            self.seen[eng][key] = last
            waits.append((key, last))
        ent[1] = last + 16
        return (key, last + 16)

    def emit(self, eng, fn, reads=(), writes=(), dma=False):
        deps = {}

        def add(tok):
            k, v = tok
            if deps.get(k, 0) < v:
                deps[k] = v

        rb = [b for v in reads for b in v.bufs]
        wb = [b for v in writes for b in v.bufs]
        for b in rb:
            if b.lw:
                add(b.lw)
        for b in wb:
            if b.lw:
                add(b.lw)
            for k, v in b.rd.items():
                add((k, v))
        waits = []
        for k, v in deps.items():
            if k[0] == eng and not self.same_sync[eng]:
                continue
            if self.seen[eng].get(k, 0) >= v:
                continue
            self.seen[eng][k] = v
            waits.append((k, v))
        tok = self._tok_dma(eng, waits) if dma else self._tok_engine(eng)
        self.ops[eng].append((waits, fn, tok, dma))
        for b in rb:
            if b.rd.get(tok[0], 0) < tok[1]:
                b.rd[tok[0]] = tok[1]
        for b in wb:
            b.lw = tok
            b.rd = {}
        return tok

    def barrier(self):
        toks = []
        for e in ENGS:
            if self.cnt[e] > 0:
                idx = self.cnt[e] - 1
                toks.append(((e, idx // SEM_M), idx % SEM_M + 1))
        for e, pool in self.dma_pool.items():
            for key, last in pool:
                if last > 0:
                    toks.append((key, last))
        for e in ENGS:
            waits = []
            for k, v in toks:
                if k[0] == e and k[0] != "dma" and not self.same_sync[e]:
                    pass
                if self.seen[e].get(k, 0) >= v:
                    continue
                self.seen[e][k] = v
                waits.append((k, v))
            if waits:
                self.ops[e].append((waits, None, None, False))

    def mm(self, out, lhsT, rhs, start=True, stop=True):
        o, l, r = out.ap, lhsT.ap, rhs.ap
        self.emit("pe", lambda e: e.matmul(o, l, r, start=start, stop=stop), reads=(lhsT, rhs), writes=(out,))

    def transpose(self, out, in_, ident):
        o, i, d = out.ap, in_.ap, ident.ap
        self.emit("pe", lambda e: e.transpose(o, i, d), reads=(in_, ident), writes=(out,))

    def tt(self, eng, out, in0, in1, op):
        o, a, b = out.ap, in0.ap, in1.ap
        self.emit(eng, lambda e: e.tensor_tensor(o, a, b, op), reads=(in0, in1), writes=(out,))

    def ts(self, eng, out, in0, s1, s2, op0, op1=None):
        o, a = out.ap, in0.ap
        reads = [in0]
        if isinstance(s1, View):
            reads.append(s1)
            s1 = s1.ap
        if isinstance(s2, View):
            reads.append(s2)
            s2 = s2.ap
        if op1 is None:
            self.emit(eng, lambda e: e.tensor_scalar(o, a, s1, None, op0), reads=reads, writes=(out,))
        else:
            self.emit(eng, lambda e: e.tensor_scalar(o, a, s1, s2, op0, op1), reads=reads, writes=(out,))

    def stt(self, eng, out, in0, scalar, in1, op0, op1):
        o, a, b = out.ap, in0.ap, in1.ap
        reads = [in0, in1]
        if isinstance(scalar, View):
            reads.append(scalar)
            scalar = scalar.ap
        self.emit(eng, lambda e: e.scalar_tensor_tensor(o, a, scalar, b, op0, op1), reads=reads, writes=(out,))

    def copy(self, eng, out, in_):
        o, a = out.ap, in_.ap
        if eng == "act":
            self.emit(eng, lambda e: e.copy(o, a), reads=(in_,), writes=(out,))
        else:
            self.emit(eng, lambda e: e.tensor_copy(o, a), reads=(in_,), writes=(out,))

    def act(self, out, in_, func, bias=None, scale=None, accum_out=None):
        o, a = out.ap, in_.ap
        reads = [in_]
        writes = [out]
        kw = {}
        if bias is not None:
            if isinstance(bias, View):
                reads.append(bias)
                bias = bias.ap
            kw["bias"] = bias
        if scale is not None:
            if isinstance(scale, View):
                reads.append(scale)
                scale = scale.ap
            kw["scale"] = scale
        if accum_out is not None:
            writes.append(accum_out)
            kw["accum_out"] = accum_out.ap
        self.emit("act", lambda e: e.activation(o, a, func, **kw), reads=reads, writes=writes)

    def recip(self, out, in_):
        o, a = out.ap, in_.ap
        self.emit("dve", lambda e: e.reciprocal(o, a), reads=(in_,), writes=(out,))

    def vmax(self, out, in_):
        o, a = out.ap, in_.ap
        self.emit("dve", lambda e: e.max(o, a), reads=(in_,), writes=(out,))

    def memset(self, eng, out, val):
        o = out.ap
        self.emit(eng, lambda e: e.memset(o, val), writes=(out,))

    def scan(self, out, d0, d1, init, op0, op1):
        o, a, b = out.ap, d0.ap, d1.ap
        reads = [d0, d1]
        if isinstance(init, View):
            reads.append(init)
            init = init.ap
        self.emit("dve", lambda e: e.tensor_tensor_scan(o, a, b, init, op0, op1), reads=reads, writes=(out,))

    def dma(self, eng, out, in_, out_is_dram=False, in_is_dram=False, final=False):
        o = out if out_is_dram else out.ap
        a = in_ if in_is_dram else in_.ap
        reads = () if in_is_dram else (in_,)
        writes = () if out_is_dram else (out,)
        tok = self.emit(eng, lambda e: e.dma_start(out=o, in_=a), reads=reads, writes=writes, dma=True)
        if final:
            self.out_tokens.append(tok)
        return tok

    def ps(self):
        b = self.psb[self.ps_rr % len(self.psb)]
        self.ps_rr += 1
        return b

    def finish(self, eng="sp"):
        waits = []
        for k, v in self.out_tokens:
            if self.seen[eng].get(k, 0) < v:
                self.seen[eng][k] = v
                waits.append((k, v))
        self.ops[eng].append((waits, None, None, False))

    def replay(self):
        nc = self.nc
        with nc.Block() as block:
            def run(ename):
                def body(e):
                    for waits, fn, tok, dma in self.ops[ename]:
                        for k, v in waits:
                            e.wait_ge(self.sems[k], v)
                        if fn is not None:
                            ins = fn(e)
                            ins.then_inc(self.sems[tok[0]], 16 if dma else 1)
                return body
            block.tensor(run("pe"))
            block.scalar(run("act"))
            block.vector(run("dve"))
            block.gpsimd(run("pool"))
            block.sync(run("sp"))


class Ctx:
    pass


def load_wblock(P, eng, dst, w_dram, r0, nrows, c0, ncols):
    src = w_dram[r0:r0 + nrows, c0:c0 + ncols].rearrange("(kc p) n -> p kc n", p=128)
    P.dma(eng, dst, src, in_is_dram=True)


def layer_norm(P, C, Z, X, Xb, g_col, b_col):
    st = ExitStack()
    sq = [P.sb([128, TT], F32, "lnsq", st) for _ in range(3)]
    accS = [P.sb([128, TT], F32, "lnS", st) for _ in range(2)]
    accQ = [P.sb([128, TT], F32, "lnQ", st) for _ in range(2)]
    mean = [P.sb([128, TT], F32, "lnmean", st) for _ in range(2)]
    rstd = [P.sb([128, TT], F32, "lnrstd", st) for _ in range(2)]
    t1s = [P.sb([128, TT], F32, "lnt", st) for _ in range(3)]
    t2s = [P.sb([128, TT], F32, "lnt2", st) for _ in range(3)]

    def stats(ti):
        i2 = ti % 2
        S_, Q_ = accS[i2], accQ[i2]
        ps_m = P.ps()
        for c in range(KC):
            P.mm(ps_m.v(), C.ones_inv.v(), Z.tl(c, ti), start=(c == 0), stop=(c == KC - 1))
        for c in range(KC):
            q = sq[c % 3]
            P.act(q.v(), Z.tl(c, ti), AF.Square)
            if c == 0:
                P.copy("dve", Q_.v(), q.v())
            else:
                P.tt("dve", Q_.v(), Q_.v(), q.v(), ALU.add)
        P.copy("act", mean[i2].v(), ps_m.v())
        ps_q = P.ps()
        P.mm(ps_q.v(), C.ones_inv.v(), Q_.v())
        P.tt("dve", S_.v(), mean[i2].v(), mean[i2].v(), ALU.mult)
        P.stt("dve", rstd[i2].v(), ps_q.v(), LN_EPS, S_.v(), ALU.add, ALU.subtract)
        P.act(rstd[i2].v(), rstd[i2].v(), AF.Ln)
        P.act(rstd[i2].v(), rstd[i2].v(), AF.Exp, scale=-0.5)

    def apply(ti):
        i2 = ti % 2
        for c in range(KC):
            t1, t2 = t1s[c % 3], t2s[c % 3]
            P.tt("dve", t1.v(), Z.tl(c, ti), mean[i2].v(), ALU.subtract)
            P.tt("dve", t2.v(), t1.v(), rstd[i2].v(), ALU.mult)
            if c < 4:
                P.act(X.tl(c, ti), t2.v(), AF.Identity, bias=b_col[:, c:c + 1], scale=g_col[:, c:c + 1])
            else:
                P.ts("dve", X.tl(c, ti), t2.v(), g_col[:, c:c + 1], b_col[:, c:c + 1], ALU.mult, ALU.add)
            P.act(Xb.tl(c, ti), t2.v(), AF.Identity, bias=b_col[:, c:c + 1], scale=g_col[:, c:c + 1])

    stats(0)
    for ti in range(NT):
        if ti + 1 < NT:
            stats(ti + 1)
        apply(ti)
    st.close()
    P.barrier()


def ffn(P, C, X, Xb, experts, dff, gates=None):
    for c in range(KC):
        for ti in range(NT):
            if (c + ti) % 2 == 0:
                P.act(X.tl(c, ti), X.tl(c, ti), AF.Copy, scale=ALPHA)
            else:
                P.ts("dve", X.tl(c, ti), X.tl(c, ti), ALPHA, None, ALU.mult)
    FB = 512
    blocks = []
    for e, (wgu, wdn) in enumerate(experts):
        f0 = 0
        while f0 < dff:
            fb = min(FB, dff - f0)
            blocks.append((e, wgu, wdn, f0, fb))
            f0 += fb
    with ExitStack() as st:
        NB = 2
        wg = [P.sb([128, KC, FB], BF16, "wg", st) for _ in range(NB)]
        wu = [P.sb([128, KC, FB], BF16, "wu", st) for _ in range(NB)]
        wd = [P.sb([128, FB // 128, D], BF16, "wd", st) for _ in range(NB)]
        hb = [P.grid(FB // 128, TT, BF16, "hb", TT, st) for _ in range(2)]
        sg = [P.sb([128, TT], F32, "sg", st) for _ in range(2)]
        sg2 = [P.sb([128, TT], F32, "sg2", st) for _ in range(2)]
        scnt = [0]

        def load(bi):
            e, wgu, wdn, f0, fb = blocks[bi]
            nfc = fb // 128
            load_wblock(P, "pool", wg[bi % NB][:, :, 0:fb], wgu, 0, D, f0, fb)
            load_wblock(P, "pool", wu[bi % NB][:, :, 0:fb], wgu, 0, D, dff + f0, fb)
            load_wblock(P, "pool", wd[bi % NB][:, 0:nfc, :], wdn, f0, fb, 0, D)

        items = [(bi, ti) for bi in range(len(blocks)) for ti in range(NT)]

        def gate_up(ii):
            bi, ti = items[ii]
            e, wgu, wdn, f0, fb = blocks[bi]
            nfc = fb // 128
            wgb, wub = wg[bi % NB], wu[bi % NB]
            H = hb[ii % 2]
            for fc in range(nfc):
                pg = P.ps()
                pu = P.ps()
                for k in range(KC):
                    P.mm(pg.v(), wgb[:, k, fc * 128:(fc + 1) * 128], Xb.tl(k, ti), start=(k == 0), stop=(k == KC - 1))
                for k in range(KC):
                    P.mm(pu.v(), wub[:, k, fc * 128:(fc + 1) * 128], Xb.tl(k, ti), start=(k == 0), stop=(k == KC - 1))
                s_ = sg[scnt[0] % 2]
                s2 = sg2[scnt[0] % 2]
                scnt[0] += 1
                P.act(s_.v(), pg.v(), AF.Silu)
                if gates is not None:
                    P.tt("dve", s2.v(), s_.v(), gates(e, ti), ALU.mult)
                    P.tt("dve", H.tl(fc, 0), s2.v(), pu.v(), ALU.mult)
                else:
                    P.tt("dve", H.tl(fc, 0), s_.v(), pu.v(), ALU.mult)

        def down(ii):
            bi, ti = items[ii]
            e, wgu, wdn, f0, fb = blocks[bi]
            nfc = fb // 128
            wdb = wd[bi % NB]
            H = hb[ii % 2]
            for dc in range(KC):
                po = P.ps()
                for fc in range(nfc):
                    P.mm(po.v(), wdb[:, fc, dc * 128:(dc + 1) * 128], H.tl(fc, 0), start=(fc == 0), stop=(fc == nfc - 1))
                P.tt("dve", X.tl(dc, ti), X.tl(dc, ti), po.v(), ALU.add)

        load(0)
        if gates is not None:
            gates(blocks[0][0], 0)
        gate_up(0)
        for ii in range(len(items)):
            bi, ti = items[ii]
            if ti == 0 and bi + 1 < len(blocks):
                load(bi + 1)
            if ii + 1 < len(items):
                gate_up(ii + 1)
            down(ii)
    P.barrier()


def moe(P, C, X, Xb, w_router, w_gu, w_down):
    with ExitStack() as st:
        wr = P.sb([128, KC, NE], F32, "wr", st)
        P.dma("sp", wr.v(), w_router.rearrange("(kc p) e -> p kc e", p=128), in_is_dram=True)
        GT = P.sb([NE, S], F32, "GT", st)
        C.sel = P.sb([NE, NE * 128], F32, "sel", st)
        P.dma("sp", C.sel.v(), C.dr["c_sel"], in_is_dram=True)
        lg = [P.sb([128, NE], F32, "lg", st) for _ in range(2)]
        mx = [P.sb([128, 8], F32, "mx", st) for _ in range(2)]
        sm = [P.sb([128, 4], F32, "sm", st) for _ in range(2)]
        e1 = [P.sb([128, NE], F32, "e1", st) for _ in range(2)]
        mk = [P.sb([128, NE], F32, "mk", st) for _ in range(2)]
        for tt_ in range(S // 128):
            pl = P.ps()
            for k in range(KC):
                P.mm(pl[:, 0:NE], X.v(k, tt_ * 128, (tt_ + 1) * 128), wr[:, k, :], start=(k == 0), stop=(k == KC - 1))
            L, M, Sm, E1, MK = lg[tt_ % 2], mx[tt_ % 2], sm[tt_ % 2], e1[tt_ % 2], mk[tt_ % 2]
            P.copy("dve", L.v(), pl[:, 0:NE])
            P.vmax(M.v(), L.v())
            P.ts("dve", Sm[:, 0:1], M[:, 0:1], -1.0, None, ALU.mult)
            P.act(Sm[:, 1:2], M[:, 1:2], AF.Exp, bias=Sm[:, 0:1])
            P.ts("dve", Sm[:, 2:3], Sm[:, 1:2], 1.0, None, ALU.add)
            P.recip(Sm[:, 2:3], Sm[:, 2:3])
            P.act(E1.v(), L.v(), AF.Exp, bias=Sm[:, 0:1])
            P.ts("dve", MK.v(), L.v(), M[:, 1:2], None, ALU.is_ge)
            P.tt("dve", MK.v(), MK.v(), E1.v(), ALU.mult)
            P.ts("dve", MK.v(), MK.v(), Sm[:, 2:3], None, ALU.mult)
            pt = P.ps()
            P.transpose(pt[0:NE, 0:128], MK.v(), C.ident.v())
            P.copy("act", GT[:, tt_ * 128:(tt_ + 1) * 128], pt[0:NE, 0:128])
        gbc = [P.grid(1, S, F32, "gbc", TT, st) for _ in range(2)]
        state = {"e": -1}

        def gates(e, ti):
            if state["e"] != e:
                state["e"] = e
                for t2 in range(NT):
                    pb = P.ps()
                    P.mm(pb.v(), C.sel[:, e * 128:(e + 1) * 128], GT[:, t2 * TT:(t2 + 1) * TT])
                    P.copy("act", gbc[e % 2].tl(0, t2), pb.v())
            return gbc[e % 2].tl(0, ti)

        ffn(P, C, X, Xb, [(w_gu[e], w_down[e]) for e in range(NE)], D_FFE, gates=gates)


def mixer_b(P, C, X, Xb, dr):
    RT = 256
    NRT = S // RT
    NCH = RT // 64
    Y = Xb
    with ExitStack() as st:
        prm = P.sb([128, 8, 12], F32, "bprm", st)
        P.dma("sp", prm[:, :, 0:11], dr["b_prm"], in_is_dram=True)
        P.ts("dve", prm[:, :, 11:12], prm[:, :, 9:10], -1.0, 1.0, ALU.mult, ALU.add)
        w1 = P.sb([128, KC, 64], BF16, "w1", st)
        a1 = P.sb([128, KC, 64], BF16, "a1", st)
        g1 = P.sb([128, KC, 160], BF16, "g1", st)
        load_wblock(P, "pool", w1.v(), dr["b_w1"], 0, D, 0, 64)
        load_wblock(P, "pool", a1.v(), dr["b_a1"], 0, D, 0, 64)
        load_wblock(P, "pool", g1.v(), dr["b_g1"], 0, D, 0, 160)
        w2 = P.sb([64, D], BF16, "w2", st)
        a2 = P.sb([64, D], BF16, "a2", st)
        g2a = P.sb([128, D], BF16, "g2a", st)
        g2b = P.sb([32, D], BF16, "g2b", st)
        P.dma("pool", w2.v(), dr["b_w2"], in_is_dram=True)
        P.dma("pool", a2.v(), dr["b_a2"], in_is_dram=True)
        P.dma("pool", g2a.v(), dr["b_g2"][0:128, :], in_is_dram=True)
        P.dma("pool", g2b.v(), dr["b_g2"][128:160, :], in_is_dram=True)
        msk = P.sb([128, 3, 128], F32, "bmask", st)
        P.dma("sp", msk.v(), dr["c_b_mask"], in_is_dram=True)
        bones = P.sb([128, 128], F32, "bones", st)
        P.dma("sp", bones.v(), dr["c_b_ones"], in_is_dram=True)
        identb = P.sb([128, 128], BF16, "identb", st)
        P.copy("dve", identb.v(), C.ident.v())
        onec = P.sb([128, 1], BF16, "onec", st)
        P.memset("dve", onec.v(), 1.0)
        cmask = P.sb([128, RT], F32, "cmask", st)
        P.memset("dve", cmask.v(), 1.0)
        P.memset("dve", cmask.v().re("p (c s) -> p c s", s=64)[:, :, 0:1], 0.0)
        H = P.grid(8, 128, F32, "H", 128, st)
        Hb = P.grid(8, 128, BF16, "Hb", 128, st)
        for hp in range(8):
            P.memset("pool", H.tl(hp, 0), 0.0)
            P.memset("pool", Hb.tl(hp, 0), 0.0)
        xx = P.grid(KC, RT, F32, "xx", RT, st)
        xj = [P.grid(KC, RT, BF16, "xj", RT, st) for _ in range(6)]
        twb = P.sb([64, RT], BF16, "twb", st)
        tab = P.sb([64, RT], BF16, "tab", st)
        tgb0 = P.sb([128, RT], BF16, "tgb0", st)
        tgb1 = P.sb([32, RT], BF16, "tgb1", st)
        wrkv = [P.sb([128, KC, 128], BF16, "wrkv", st) for _ in range(3)]

        def F(nm, dt=F32):
            return P.sb([128, RT], dt, nm, st)
        sg, Lc, Lp, EPi, EPp, asig, k_, r_, kk, t1, t2, t3, k2 = (F(n) for n in ("sg", "Lc", "Lp", "EPi", "EPp", "asig", "k_", "r_", "kk", "t1", "t2", "t3", "k2"))
        t4, t5 = Lp, Lc
        EP2 = [F("EP"), F("EP")]
        Gt2 = [F("Gt"), F("Gt")]
        gn2 = [P.sb([128, 128], F32, "gn2", st) for _ in range(2)]
        EXP2 = []
        for bs_ in range(2):
            EXP = {}
            for nm in ("A", "B", "K", "R", "V", "Q"):
                EXP[nm] = P.grid(NCH, 128, BF16, "E" + nm, 128, st)
                for c in range(NCH):
                    P.memset("pool", EXP[nm].tl(c, 0), 0.0)
            EXP2.append(EXP)
        yc4 = P.sb([128, NCH, 64], F32, "yc4", st)
        yq4 = P.sb([128, NCH, 64], F32, "yq4", st)
        st4 = P.sb([128, 2, NCH], F32, "st4", st)
        sb4 = P.sb([128, NCH], F32, "sb4", st)
        NPOOL = 32
        sq_pool = [P.sb([128, 128], BF16, "sq", st) for _ in range(NPOOL)]
        sqi = [0]

        def SQ():
            t = sq_pool[sqi[0] % NPOOL]
            sqi[0] += 1
            return t
        fixed = {nm: [P.sb([128, 128], BF16, nm, st) for _ in range(2 if nm in ("Wb", "Ub") else NCH)] * (2 if nm in ("Wb", "Ub") else 1) for nm in ("VEt", "BEt", "KEt", "AKt", "RBt", "RKt", "Wb", "Ub", "YE")}
        for t in fixed["YE"]:
            P.memset("pool", t.v(), 0.0)
        cc = 0

        def half_expand(eng, E, fn):
            for hh in range(2):
                rows = slice(hh * 64, hh * 64 + 64)
                outv = View([b for c in range(NCH) for b in E.bufs[c]], E.t[rows, :, hh * 64:hh * 64 + 64])
                fn(rows, outv)

        def v3(view, rows):
            return view[rows, :].re("p (c s) -> p c s", s=64)

        for rt in range(NRT):
            t0 = rt * RT
            for kc in range(KC):
                P.tt("pool", xx.v(kc, 1, RT), X.v(kc, t0, t0 + RT - 1), X.v(kc, t0 + 1, t0 + RT), ALU.subtract)
                if rt == 0:
                    P.ts("dve", xx.v(kc, 0, 1), X.v(kc, 0, 1), -1.0, None, ALU.mult)
                else:
                    P.tt("dve", xx.v(kc, 0, 1), X.v(kc, t0 - 1, t0), X.v(kc, t0, t0 + 1), ALU.subtract)
                for j in range(6):
                    P.stt("dve", xj[j].tl(kc, 0), xx.tl(kc, 0), prm[:, kc, j:j + 1], X.v(kc, t0, t0 + RT), ALU.mult, ALU.add)
            pw = P.ps()
            for kc in range(KC):
                P.mm(pw[0:64, 0:RT], w1[:, kc, :], xj[1].tl(kc, 0), start=(kc == 0), stop=(kc == KC - 1))
            P.act(twb.v(), pw[0:64, 0:RT], AF.Tanh)
            pa = P.ps()
            for kc in range(KC):
                P.mm(pa[0:64, 0:RT], a1[:, kc, :], xj[4].tl(kc, 0), start=(kc == 0), stop=(kc == KC - 1))
            P.copy("act", tab.v(), pa[0:64, 0:RT])
            pg0 = P.ps()
            for kc in range(KC):
                P.mm(pg0[:, 0:RT], g1[:, kc, 0:128], xj[5].tl(kc, 0), start=(kc == 0), stop=(kc == KC - 1))
            P.act(tgb0.v(), pg0[:, 0:RT], AF.Sigmoid)
            pg1 = P.ps()
            for kc in range(KC):
                P.mm(pg1[0:32, 0:RT], g1[:, kc, 128:160], xj[5].tl(kc, 0), start=(kc == 0), stop=(kc == KC - 1))
            P.act(tgb1.v(), pg1[0:32, 0:RT], AF.Sigmoid)
            def load_w(hp):
                for i3, nm in enumerate(("b_w_r", "b_w_k", "b_w_v")):
                    load_wblock(P, "pool", wrkv[i3].v(), dr[nm], 0, D, hp * 128, 128)

            def pair_prep(hp):
                bs = hp % 2
                cols = slice(hp * 128, (hp + 1) * 128)
                EPb, Gtb = EP2[bs], Gt2[bs]
                EX = {n: EXP2[bs][n] for n in EXP2[bs]}
                P.dma("sp", gn2[bs].v(), dr["b_gn_tok"][:, hp, :], in_is_dram=True)
                if hp == 0:
                    load_w(0)
                pr, pk, pv = P.ps(), P.ps(), P.ps()
                for pp, wt, xi in ((pr, wrkv[0], 0), (pk, wrkv[1], 2), (pv, wrkv[2], 3)):
                    for kc in range(KC):
                        P.mm(pp[:, 0:RT], wt[:, kc, :], xj[xi].tl(kc, 0), start=(kc == 0), stop=(kc == KC - 1))
                P.copy("act", k_.v(), pk[:, 0:RT])
                P.copy("act", r_.v(), pr[:, 0:RT])
                half_expand("act", EX["V"], lambda rows, outv: P.copy("act", outv, v3(pv[:, 0:RT], rows)))
                yield
                if hp < 7:
                    load_w(hp + 1)
                pwl, pal, pgl = P.ps(), P.ps(), P.ps()
                P.mm(pwl[:, 0:RT], w2[:, cols], twb.v())
                P.mm(pal[:, 0:RT], a2[:, cols], tab.v())
                P.mm(pgl[:, 0:RT], g2a[:, cols], tgb0.v(), start=True, stop=False)
                P.mm(pgl[:, 0:RT], g2b[:, cols], tgb1.v(), start=False, stop=True)
                P.act(sg.v(), pwl[:, 0:RT], AF.Sigmoid, bias=prm[:, hp, 6:7])
                P.act(asig.v(), pal[:, 0:RT], AF.Sigmoid, bias=prm[:, hp, 7:8])
                P.copy("act", Gtb.v(), pgl[:, 0:RT])
                yield
                P.ts("dve", sg.v(), sg.v(), -math.exp(-0.5), None, ALU.mult)
                yield
                P.scan(Lc.v(), cmask.v(), sg.v(), 0.0, ALU.mult, ALU.add)
                yield
                P.ts("dve", kk.v(), k_.v(), prm[:, hp, 8:9], None, ALU.mult)
                yield
                P.tt("dve", t1.v(), kk.v(), kk.v(), ALU.mult)
                yield
                pss = P.ps()
                P.mm(pss[:, 0:RT], bones.v(), t1.v())
                P.ts("dve", t1.v(), pss[:, 0:RT], 1e-24, None, ALU.max)
                P.tt("pool", Lp.v(), Lc.v(), sg.v(), ALU.subtract)
                yield
                P.act(EPb.v(), Lc.v(), AF.Exp)
                P.act(EPi.v(), Lc.v(), AF.Exp, scale=-1.0)
                yield
                P.act(EPp.v(), Lp.v(), AF.Exp)
                yield
                P.act(t1.v(), t1.v(), AF.Ln)
                P.ts("dve", t2.v(), asig.v(), prm[:, hp, 9:10], prm[:, hp, 11:12], ALU.mult, ALU.add)
                yield
                P.act(t1.v(), t1.v(), AF.Exp, scale=-0.5)
                P.tt("dve", k2.v(), k_.v(), t2.v(), ALU.mult)
                yield
                P.tt("dve", kk.v(), kk.v(), t1.v(), ALU.mult)
                yield
                P.tt("dve", t2.v(), r_.v(), EPb.v(), ALU.mult)
                yield
                half_expand("act", EX["R"], lambda rows, outv: P.copy("act", outv, v3(t2.v(), rows)))
                P.stt("dve", t1.v(), kk.v(), -1.0, EPp.v(), ALU.mult, ALU.mult)
                yield
                half_expand("pool", EX["A"], lambda rows, outv: P.copy("pool", outv, v3(t1.v(), rows)))
                P.tt("dve", t3.v(), kk.v(), asig.v(), ALU.mult)
                yield
                P.tt("dve", t3.v(), t3.v(), EPi.v(), ALU.mult)
                yield
                half_expand("act", EX["B"], lambda rows, outv: P.copy("act", outv, v3(t3.v(), rows)))
                P.tt("dve", t4.v(), k2.v(), EPi.v(), ALU.mult)
                yield
                half_expand("pool", EX["K"], lambda rows, outv: P.copy("pool", outv, v3(t4.v(), rows)))
                P.stt("dve", t5.v(), r_.v(), prm[:, hp, 10:11], k2.v(), ALU.mult, ALU.mult)
                yield
                half_expand("act", EX["Q"], lambda rows, outv: P.copy("act", outv, v3(t5.v(), rows)))
                yield

            def pair_chunks(hp, t0):
                bs = hp % 2
                EPb, Gtb, gnb = EP2[bs], Gt2[bs], gn2[bs]
                EX = EXP2[bs]
                CH = []
                for c in range(NCH):
                    d_ = {}
                    d_["EA"], d_["EB"], d_["EK"], d_["ER"], d_["EV"], d_["EQ"] = (EX[n].tl(c, 0) for n in ("A", "B", "K", "R", "V", "Q"))
                    for n in ("VEt", "BEt", "KEt", "AKt", "RBt", "RKt", "Wb", "Ub", "YE"):
                        d_[n] = fixed[n][c]
                    CH.append(d_)
                for c in range(NCH):
                    d_ = CH[c]
                    for src, dst in ((d_["EV"], d_["VEt"]), (d_["EB"], d_["BEt"]), (d_["EK"], d_["KEt"])):
                        ptt = P.ps().v().cast(BF16)
                        P.transpose(ptt[:, 0:128], src, identb.v())
                        P.copy("act", dst.v(), ptt[:, 0:128])
                        yield
                for c in range(NCH):
                    d_ = CH[c]
                    Nm, Lm, T, Tt = SQ(), SQ(), SQ(), SQ()
                    p_ = P.ps()
                    P.mm(p_[:, 0:128], d_["EB"], d_["EA"])
                    P.tt("dve", Nm.v(), p_[:, 0:128], msk[:, 0, :], ALU.mult)
                    P.tt("pool", T.v(), Nm.v(), identb.v(), ALU.add)
                    yield
                    p_ = P.ps()
                    P.mm(p_[:, 0:128], d_["EA"], d_["EB"])
                    P.tt("dve", Lm.v(), p_[:, 0:128], msk[:, 1, :], ALU.mult)
                    P.tt("pool", Tt.v(), Lm.v(), identb.v(), ALU.add)
                    d_["N"], d_["L"], d_["T"], d_["Tt"] = Nm, Lm, T, Tt
                    yield
                for c in range(NCH):
                    d_ = CH[c]
                    for (l_, r_op, dst, mi) in ((d_["EK"], d_["EA"], d_["AKt"], 0), (d_["EB"], d_["ER"], d_["RBt"], 2), (d_["EK"], d_["ER"], d_["RKt"], 2)):
                        p_ = P.ps()
                        P.mm(p_[:, 0:128], l_, r_op)
                        P.tt("dve", dst.v(), p_[:, 0:128], msk[:, mi, :], ALU.mult)
                        yield
                for lvl in range(5):
                    last = (lvl == 4)
                    for c in range(NCH):
                        d_ = CH[c]
                        N2 = SQ()
                        p_ = P.ps()
                        P.mm(p_[:, 0:128], d_["L"].v(), d_["N"].v())
                        P.copy("act", N2.v(), p_[:, 0:128])
                        d_["N2"] = N2
                        yield
                        if not last:
                            L2 = SQ()
                            p_ = P.ps()
                            P.mm(p_[:, 0:128], d_["N"].v(), d_["L"].v())
                            P.copy("act", L2.v(), p_[:, 0:128])
                            d_["L2"] = L2
                            yield
                    for c in range(NCH):
                        d_ = CH[c]
                        Tn = SQ()
                        p_ = P.ps()
                        P.mm(p_[:, 0:128], d_["Tt"].v(), d_["N2"].v())
                        P.tt("dve", Tn.v(), p_[:, 0:128], d_["T"].v(), ALU.add)
                        yield
                        if not last:
                            Ttn = SQ()
                            p_ = P.ps()
                            P.mm(p_[:, 0:128], d_["T"].v(), d_["L2"].v())
                            P.tt("dve", Ttn.v(), p_[:, 0:128], d_["Tt"].v(), ALU.add)
                            d_["N"], d_["L"], d_["T"], d_["Tt"] = d_["N2"], d_["L2"], Tn, Ttn
                            yield
                        else:
                            d_["T"] = Tn
                Hbv = Hb.tl(hp, 0)
                Hv = H.tl(hp, 0)
                yield "C"
                pb4 = P.ps()
                for c in range(NCH):
                    P.mm(pb4[:, c:c + 1], CH[c]["EQ"], onec.v())
                P.copy("act", sb4.v(), pb4[:, 0:NCH])
                yield
                for c in range(NCH):
                    d_ = CH[c]
                    p_ = P.ps()
                    P.mm(p_[:, 0:128], d_["EA"], Hbv, start=True, stop=False)
                    P.mm(p_[:, 0:128], d_["AKt"].v(), d_["VEt"].v(), start=False, stop=True)
                    P.copy("act", d_["Wb"].v(), p_[:, 0:128])
                    yield
                    p_ = P.ps()
                    P.mm(p_[:, 0:128], d_["T"].v(), d_["Wb"].v())
                    P.copy("act", d_["Ub"].v(), p_[:, 0:128])
                    yield
                    pst_ = P.ps()
                    P.mm(pst_[:, 0:128], d_["BEt"].v(), d_["Ub"].v(), start=True, stop=False)
                    P.mm(pst_[:, 0:128], d_["KEt"].v(), d_["VEt"].v(), start=False, stop=True)
                    py = P.ps()
                    P.mm(py[:, 0:128], d_["ER"], Hbv, start=True, stop=False)
                    P.mm(py[:, 0:128], d_["RBt"].v(), d_["Ub"].v(), start=False, stop=False)
                    P.mm(py[:, 0:128], d_["RKt"].v(), d_["VEt"].v(), start=False, stop=True)
                    P.tt("dve", Hv, Hv, pst_[:, 0:128], ALU.add)
                    P.copy("act", yc4[0:64, c, :], py[0:64, 0:64])
                    P.copy("act", yc4[64:128, c, :], py[64:128, 64:128])
                    yield
                    P.ts("dve", Hv, Hv, EPb[:, c * 64 + 63:c * 64 + 64], None, ALU.mult)
                    yield
                    P.copy("act", Hbv, Hv)
                    yield
                def red(out, in_):
                    o, a = out.ap, in_.ap
                    P.emit("dve", lambda e: e.reduce_sum(o, a, AX.X), reads=(in_,), writes=(out,))
                red(st4[:, 0, :], yc4.v())
                P.ts("dve", st4[:, 0, :], st4[:, 0, :], -1.0 / 64.0, None, ALU.mult)
                yield
                P.tt("dve", yc4.v(), yc4.v(), st4[:, 0, :].re("p (c o) -> p c o", o=1).bc([128, NCH, 64]), ALU.add)
                yield
                P.tt("pool", yq4.v(), yc4.v(), yc4.v(), ALU.mult)
                yield
                red(st4[:, 1, :], yq4.v())
                P.ts("dve", st4[:, 1, :], st4[:, 1, :], 1.0 / 64.0, 64e-5, ALU.mult, ALU.add)
                yield
                P.act(st4[:, 1, :], st4[:, 1, :], AF.Ln)
                P.act(st4[:, 1, :], st4[:, 1, :], AF.Exp, scale=-0.5)
                yield
                P.tt("dve", yc4.v(), yc4.v(), st4[:, 1, :].re("p (c o) -> p c o", o=1).bc([128, NCH, 64]), ALU.mult)
                yield
                P.tt("dve", yc4.v(), yc4.v(), gnb[:, 0:64].re("p (o v) -> p o v", o=1).bc([128, NCH, 64]), ALU.mult)
                yield
                P.tt("dve", yc4.v(), yc4.v(), gnb[:, 64:128].re("p (o v) -> p o v", o=1).bc([128, NCH, 64]), ALU.add)
                yield
                for c in range(NCH):
                    d_ = CH[c]
                    for hh in range(2):
                        rows = slice(hh * 64, hh * 64 + 64)
                        cb = slice(hh * 64, hh * 64 + 64)
                        P.stt("dve", d_["YE"][rows, cb], d_["VEt"][rows, cb], sb4[rows, c:c + 1], yc4[rows, c, :], ALU.mult, ALU.add)
                    yield
                for c in range(NCH):
                    d_ = CH[c]
                    tok0 = t0 + c * 64
                    ptt = P.ps().v().cast(BF16)
                    P.transpose(ptt[:, 0:128], d_["YE"].v(), identb.v())
                    for hh in range(2):
                        rows = slice(hh * 64, hh * 64 + 64)
                        cb = slice(hh * 64, hh * 64 + 64)
                        P.tt("dve", Y.v(hp, tok0, tok0 + 64)[rows, :], ptt[rows, cb], Gtb[rows, c * 64:(c + 1) * 64], ALU.mult)
                    yield

            def drive(ga, gb, ratio):
                da = ga is None
                db = gb is None
                while not (da and db):
                    if not da:
                        try:
                            next(ga)
                        except StopIteration:
                            da = True
                    for _ in range(ratio):
                        if db:
                            break
                        try:
                            next(gb)
                        except StopIteration:
                            db = True

            prev = None
            for hp in range(8):
                if INTERLEAVE:
                    drive(pair_prep(hp), pair_chunks(prev, t0) if prev is not None else None, DRIVE_RATIO)
                    prev = hp
                else:
                    drive(pair_prep(hp), None, 1)
                    drive(None, pair_chunks(hp, t0), 1)
            if INTERLEAVE:
                drive(None, pair_chunks(prev, t0), 1)
        st_out = st.enter_context(ExitStack())
    P.barrier()
    with ExitStack() as st:
        out_proj(P, C, X, Y, KC, dr["b_w_o"], st)
    P.barrier()


def load_posf(P, dr, st):
    posf = P.grid(1, S, F32, "posf", TT, st)
    posi = P.grid(1, S, I32, "posi", TT, st)
    src = dr["pos"].to_broadcast([128, S])
    for ti in range(NT):
        P.dma("sp", posi.tl(0, ti), src[:, ti * TT:(ti + 1) * TT], in_is_dram=True)
        P.copy("dve", posf.tl(0, ti), posi.tl(0, ti))
    return posf


INTERLEAVE = True
DRIVE_RATIO = 4
RET_G = [1.0 - 2.0 ** (-5.0 - h) for h in range(4)]


def mixer_d(P, C, X, Xb, dr):
    w_in, w_o = dr["d_w_in"], dr["d_w_o"]
    with ExitStack() as st:
        cs = P.grid(2, S, F32, "cossin", TT, st)
        with ExitStack() as st2:
            posf = load_posf(P, dr, st2)
            freq = P.sb([128, 1], F32, "freq", st2)
            P.dma("sp", freq.v(), dr["c_freq"], in_is_dram=True)
            a1 = [P.sb([128, TT], F32, "ang", st2) for _ in range(2)]
            ai = [P.sb([128, TT], I32, "angi", st2) for _ in range(2)]
            af = [P.sb([128, TT], F32, "angf", st2) for _ in range(2)]
            am = [P.sb([128, TT], F32, "angm", st2) for _ in range(2)]
            for ti in range(NT):
                for which, shift in ((1, 0.0), (0, 0.25)):
                    a, n_i, n_f, m_ = a1[which], ai[which], af[which], am[which]
                    P.ts("dve", a.v(), posf.tl(0, ti), freq[:, 0:1], shift, ALU.mult, ALU.add)
                    P.copy("dve", n_i.v(), a.v())
                    P.copy("dve", n_f.v(), n_i.v())
                    P.tt("dve", a.v(), a.v(), n_f.v(), ALU.subtract)
                    P.ts("dve", m_.v(), a.v(), 0.5, None, ALU.is_ge)
                    P.tt("dve", a.v(), a.v(), m_.v(), ALU.subtract)
                    P.ts("dve", m_.v(), a.v(), -0.5, None, ALU.is_lt)
                    P.tt("dve", a.v(), a.v(), m_.v(), ALU.add)
                    P.act(cs.tl(which, ti), a.v(), AF.Sin, scale=2.0 * math.pi)
        P.barrier()
        cm = P.sb([128, 4, 128], F32, "rmask", st)
        qdc = P.sb([128, 4, 128], F32, "rqd", st)
        kic = P.sb([128, 4, 128], F32, "rki", st)
        P.dma("sp", cm.v(), dr["c_ret_mask"], in_is_dram=True)
        P.dma("sp", qdc.v(), dr["c_ret_qd"], in_is_dram=True)
        P.dma("sp", kic.v(), dr["c_ret_ki"], in_is_dram=True)
        identb = P.sb([128, 128], BF16, "identb", st)
        P.copy("dve", identb.v(), C.ident.v())
        QD = P.grid(2, S, BF16, "QD", TT, st)
        KI = P.grid(2, S, BF16, "KI", TT, st)
        wqk = [P.sb([128, KC, 256], BF16, "wqk", st) for _ in range(2)]
        wv = P.sb([128, KC, 512], BF16, "wv", st)
        wg = P.sb([128, KC, 512], BF16, "wg", st)
        wo = P.sb([128, 4, D], BF16, "wo_d", st)
        YG = [P.grid(4, TT, BF16, "YG", TT, st) for _ in range(2)]
        Sst = P.sb([128, 2, 512], F32, "Sst", st)
        Sbf = P.sb([128, 2, 512], BF16, "Sbf", st)
        rt = [P.sb([128, TT], F32, "rt", st) for _ in range(4)]
        STb = [P.sb([128, 128], BF16, "STb", st) for _ in range(2)]
        KIt = [P.sb([128, 256], BF16, "KIt", st) for _ in range(2)]
        Vb = [P.sb([128, 512], BF16, "Vb", st) for _ in range(2)]
        sgl = [P.sb([128, 512], F32, "sgl", st) for _ in range(3)]
        yn = [P.sb([128, 512], F32, "yn", st) for _ in range(2)]
        ygb = [P.sb([128, 512], BF16, "ygb", st) for _ in range(2)]
        ssq = [P.sb([128, 2], F32, "ssq", st) for _ in range(2)]
        junk = P.sb([128, 512], F32, "junk", st)
        ygc = 0
        for h in range(4):
            g128 = RET_G[h] ** 128
            load_wblock(P, "pool", wv.v(), w_in, 0, D, 2 * D + h * 512, 512)
            load_wblock(P, "pool", wg.v(), w_in, 0, D, 4 * D + h * 512, 512)
            load_wblock(P, "pool", wo.v(), w_o, h * 512, 512, 0, D)
            for qk in range(2):
                wb = wqk[qk]
                load_wblock(P, "pool", wb.v(), w_in, 0, D, qk * D + h * 256, 256)
                dst = QD if qk == 0 else KI
                fac = qdc if qk == 0 else kic
                for ti in range(NT):
                    p1, p2 = P.ps(), P.ps()
                    for c, pp in enumerate((p1, p2)):
                        for k in range(KC):
                            P.mm(pp.v(), wb[:, k, c * 128:(c + 1) * 128], Xb.tl(k, ti), start=(k == 0), stop=(k == KC - 1))
                    cos, sin = cs.tl(0, ti), cs.tl(1, ti)
                    P.tt("dve", rt[0].v(), cos, p1.v(), ALU.mult)
                    P.tt("dve", rt[1].v(), sin, p2.v(), ALU.mult)
                    P.tt("pool", rt[0].v(), rt[0].v(), rt[1].v(), ALU.subtract)
                    P.tt("dve", rt[2].v(), sin, p1.v(), ALU.mult)
                    P.tt("dve", rt[3].v(), cos, p2.v(), ALU.mult)
                    P.tt("pool", rt[2].v(), rt[2].v(), rt[3].v(), ALU.add)
                    fb = fac[:, h:h + 1, :].bc([128, 4, 128])
                    P.tt("pool", dst.tl(0, ti).re("p (a b) -> p a b", b=128), rt[0].v().re("p (a b) -> p a b", b=128), fb, ALU.mult)
                    P.tt("dve", dst.tl(1, ti).re("p (a b) -> p a b", b=128), rt[2].v().re("p (a b) -> p a b", b=128), fb, ALU.mult)
            P.memset("pool", Sst.v(), 0.0)
            P.memset("pool", Sbf.v(), 0.0)
            NM = S // 128

            def stage_a(m):
                t0 = m * 128
                i2 = m % 2
                pss = P.ps()
                for dc in range(2):
                    P.mm(pss[:, 0:128], KI.v(dc, t0, t0 + 128), QD.v(dc, t0, t0 + 128), start=(dc == 0), stop=(dc == 1))
                P.tt("dve", STb[i2].v(), pss[:, 0:128], cm[:, h, :], ALU.mult)
                pv, pg = P.ps(), P.ps()
                for k in range(KC):
                    P.mm(pv.v(), Xb.v(k, t0, t0 + 128), wv[:, k, :], start=(k == 0), stop=(k == KC - 1))
                for k in range(KC):
                    P.mm(pg.v(), Xb.v(k, t0, t0 + 128), wg[:, k, :], start=(k == 0), stop=(k == KC - 1))
                P.copy("act", Vb[i2].v(), pv.v())
                P.act(sgl[m % 3].v(), pg.v(), AF.Silu)
                for dc in range(2):
                    ptt = P.ps()
                    pv_bf = ptt.v().cast(BF16)
                    P.transpose(pv_bf[:, 0:128], KI.v(dc, t0, t0 + 128), identb.v())
                    P.copy("dve", KIt[i2][:, dc * 128:(dc + 1) * 128], pv_bf[:, 0:128])

            def stage_b(m):
                t0 = m * 128
                i2 = m % 2
                po = P.ps()
                P.mm(po.v(), STb[i2].v(), Vb[i2].v(), start=True, stop=False)
                for dc in range(2):
                    P.mm(po.v(), QD.v(dc, t0, t0 + 128), Sbf[:, dc, :], start=False, stop=(dc == 1))
                for dc in range(2):
                    pk = P.ps()
                    P.mm(pk.v(), KIt[i2][:, dc * 128:(dc + 1) * 128], Vb[i2].v())
                    P.tt("dve", Sst[:, dc, :], Sst[:, dc, :], pk.v(), ALU.add)
                P.act(Sst.v(), Sst.v(), AF.Copy, scale=g128)
                P.copy("act", Sbf.v(), Sst.v())
                P.copy("act", yn[i2].v(), po.v())

            def stage_c(m):
                nonlocal ygc
                i2 = m % 2
                ti = m // 4
                Y = YG[ygc % 2]
                P.memset("pool", ssq[i2].v(), 0.0)
                P.act(junk.v(), yn[i2].v(), AF.Square, accum_out=ssq[i2][:, 0:1])
                P.ts("dve", ssq[i2][:, 1:2], ssq[i2][:, 0:1], 1.0 / 512.0, 1e-6, ALU.mult, ALU.add)
                P.act(ssq[i2][:, 1:2], ssq[i2][:, 1:2], AF.Sqrt)
                P.recip(ssq[i2][:, 1:2], ssq[i2][:, 1:2])
                P.stt("dve", ygb[i2].v(), yn[i2].v(), ssq[i2][:, 1:2], sgl[m % 3].v(), ALU.mult, ALU.mult)
                for vc in range(4):
                    ptt = P.ps()
                    pv_bf = ptt.v().cast(BF16)
                    P.transpose(pv_bf[:, 0:128], ygb[i2][:, vc * 128:(vc + 1) * 128], identb.v())
                    P.copy("act" if vc % 2 else "dve", Y.v(vc, (m % 4) * 128, (m % 4 + 1) * 128), pv_bf[:, 0:128])
                if m % 4 == 3:
                    for dc in range(KC):
                        pq = P.ps()
                        for vc in range(4):
                            P.mm(pq.v(), wo[:, vc, dc * 128:(dc + 1) * 128], Y.tl(vc, 0), start=(vc == 0), stop=(vc == 3))
                        if h == 0:
                            P.stt("dve", X.tl(dc, ti), X.tl(dc, ti), ALPHA, pq.v(), ALU.mult, ALU.add)
                        else:
                            P.tt("dve", X.tl(dc, ti), X.tl(dc, ti), pq.v(), ALU.add)
                    ygc += 1

            stage_a(0)
            for m in range(NM):
                if m + 1 < NM:
                    stage_a(m + 1)
                stage_b(m)
                if m > 0:
                    stage_c(m - 1)
            stage_c(NM - 1)
    P.barrier()


def mixer_c(P, C, X, Xb, dr):
    NB = D_RNN // 128
    with ExitStack() as st:
        Y = P.grid(NB, S, BF16, "yc", TT, st)
        st_out = st
        st = st.enter_context(ExitStack())
        prm = P.sb([128, NB, 12], F32, "cprm", st)
        P.dma("sp", prm[:, :, 0:8], dr["c_prm"], in_is_dram=True)
        P.act(prm[:, :, 9:10], prm[:, :, 7:8], AF.Exp, scale=-1.0)
        P.ts("dve", prm[:, :, 9:10], prm[:, :, 9:10], 1.0, None, ALU.add)
        P.act(prm[:, :, 10:11], prm[:, :, 9:10], AF.Ln)
        P.ts("dve", prm[:, :, 8:9], prm[:, :, 10:11], -8.0, None, ALU.mult)
        wga = P.sb([128, NB, 128], BF16, "wga", st)
        wgx = P.sb([128, NB, 128], BF16, "wgx", st)
        P.dma("pool", wga.v(), dr["c_w_gaT"], in_is_dram=True)
        P.dma("pool", wgx.v(), dr["c_w_gxT"], in_is_dram=True)
        pos_nr = P.grid(1, S, BF16, "pos_nr", TT, st)
        with ExitStack() as st2:
            posf = load_posf(P, dr, st2)
            for ti in range(NT):
                P.ts("dve", pos_nr.tl(0, ti), posf.tl(0, ti), 0.0, None, ALU.not_equal)
        P.barrier()
        wblk = [P.sb([128, KC, 256], BF16, "wc", st) for _ in range(2)]
        Upad = [P.grid(1, S + 3, F32, "upad", S + 3, st) for _ in range(2)]
        P.ts("dve", prm[:, :, 9:10], prm[:, :, 5:6], 0.5, None, ALU.mult)
        P.ts("dve", prm[:, :, 10:11], prm[:, :, 6:7], 0.5, None, ALU.mult)
        P.ts("dve", prm[:, :, 11:12], prm[:, :, 8:9], 0.5, None, ALU.mult)
        sets = []
        NSET = 3
        for _ in range(NSET):
            d_ = {}
            for nm in ("A", "I", "GG", "uc", "M", "Ht"):
                d_[nm] = P.sb([128, TT], F32, nm, st)
            sets.append(d_)
        w_in = dr["c_w_in"]

        def tile_gen(g, ti, cnt):
            B_ = sets[cnt % NSET]
            Bp = sets[(cnt - 1) % NSET]
            A, I, GG, uc, M, Ht = B_["A"], B_["I"], B_["GG"], B_["uc"], B_["M"], B_["Ht"]
            ucbv = M.v().cast(BF16)[:, 0:TT]
            wb = wblk[g % 2]
            Up = Upad[g % 2]
            if ti == 0:
                load_wblock(P, "pool", wb[:, :, 0:128], w_in, 0, D, g * 128, 128)
                load_wblock(P, "pool", wb[:, :, 128:256], w_in, 0, D, D_RNN + g * 128, 128)
                P.memset("pool", Up.v(0, 0, 3), 0.0)
            t0 = ti * TT
            pg, pu = P.ps(), P.ps()
            for k in range(KC):
                P.mm(pg.v(), wb[:, k, 0:128], Xb.tl(k, ti), start=(k == 0), stop=(k == KC - 1))
            for k in range(KC):
                P.mm(pu.v(), wb[:, k, 128:256], Xb.tl(k, ti), start=(k == 0), stop=(k == KC - 1))
            P.copy("act", Up.v(0, 3 + t0, 3 + t0 + TT), pu.v())
            P.act(A.v(), pg.v(), AF.Square)
            P.copy("act", I.v(), pg.v())
            yield
            P.ts("dve", A.v(), A.v(), 0.044715, 1.0, ALU.mult, ALU.add)
            yield
            P.tt("dve", A.v(), A.v(), I.v(), ALU.mult)
            yield
            P.act(A.v(), A.v(), AF.Tanh, scale=0.7978845608028654)
            yield
            P.stt("dve", GG.v(), A.v(), 1.0, I.v(), ALU.add, ALU.mult)
            yield
            P.ts("dve", uc.v(), Up.v(0, 3 + t0, 3 + t0 + TT), prm[:, g, 3:4], prm[:, g, 4:5], ALU.mult, ALU.add)
            yield
            for kk in range(3):
                P.stt("dve", uc.v(), Up.v(0, kk + t0, kk + t0 + TT), prm[:, g, kk:kk + 1], uc.v(), ALU.mult, ALU.add)
                yield
            P.copy("act", ucbv, uc.v())
            yield
            pr, pi = P.ps(), P.ps()
            P.mm(pr.v(), wga[:, g, :], ucbv)
            P.mm(pi.v(), wgx[:, g, :], ucbv)
            P.act(A.v(), pr.v(), AF.Tanh, bias=prm[:, g, 9:10], scale=0.5)
            P.act(I.v(), pi.v(), AF.Tanh, bias=prm[:, g, 10:11], scale=0.5)
            yield
            P.act(A.v(), A.v(), AF.Exp, bias=prm[:, g, 11:12], scale=prm[:, g, 11:12])
            yield
            P.tt("pool", A.v(), A.v(), pos_nr.tl(0, ti), ALU.mult)
            yield
            P.tt("dve", M.v(), A.v(), A.v(), ALU.mult)
            yield
            P.ts("dve", M.v(), M.v(), -1.0, 1.0, ALU.mult, ALU.add)
            yield
            P.act(M.v(), M.v(), AF.Sqrt)
            yield
            P.stt("dve", M.v(), I.v(), 1.0, M.v(), ALU.add, ALU.mult)
            yield
            P.stt("dve", M.v(), M.v(), 0.5, uc.v(), ALU.mult, ALU.mult)
            yield
            if ti == 0:
                P.scan(Ht.v(), A.v(), M.v(), 0.0, ALU.mult, ALU.add)
            else:
                P.scan(Ht.v(), A.v(), M.v(), Bp["Ht"][:, TT - 1:TT], ALU.mult, ALU.add)
            yield
            P.stt("dve", Y.tl(g, ti), GG.v(), 0.5, Ht.v(), ALU.mult, ALU.mult)
            yield

        tiles = [(g, ti) for g in range(NB) for ti in range(NT)]
        active = []
        nxt = 0
        while nxt < len(tiles) or active:
            while len(active) < NSET and nxt < len(tiles):
                g_, ti_ = tiles[nxt]
                active.append(tile_gen(g_, ti_, nxt))
                nxt += 1
            for gen in list(active):
                try:
                    next(gen)
                except StopIteration:
                    active.remove(gen)
        st.close()
        P.barrier()
        out_proj(P, C, X, Y, NB, dr["c_w_out"], st_out)
    P.barrier()


def mixer_a(P, C, X, Xb, w_in, conv_wT, w_out):
    with ExitStack() as st:
        Y = P.grid(KC, S, BF16, "ya", TT, st)
        wblk = [P.sb([128, KC, 384], BF16, "wa", st) for _ in range(2)]
        Bt = [P.grid(1, S, F32, "ab", TT, st) for _ in range(2)]
        CV = [P.grid(1, S + 2, F32, "acv", S + 2, st) for _ in range(2)]
        Ct = [P.sb([128, TT], F32, "act_c", st) for _ in range(2)]
        acc = [P.sb([128, S], F32, "aacc", st) for _ in range(2)]
        for j in range(KC):
            wb = wblk[j % 2]
            for q in range(3):
                load_wblock(P, "pool", wb[:, :, q * 128:(q + 1) * 128], w_in, 0, D, q * D + j * 128, 128)
            Bj, CVj = Bt[j % 2], CV[j % 2]
            P.memset("pool", CVj.v(0, 0, 2), 0.0)
            for ti in range(NT):
                pb, pc, pv = P.ps(), P.ps(), P.ps()
                for q, pp in enumerate((pb, pc, pv)):
                    for k in range(KC):
                        P.mm(pp.v(), wb[:, k, q * 128:(q + 1) * 128], Xb.tl(k, ti), start=(k == 0), stop=(k == KC - 1))
                P.copy("act", Bj.tl(0, ti), pb.v())
                ct = Ct[ti % 2]
                P.copy("act", ct.v(), pc.v())
                P.tt("dve", CVj.v(0, 2 + ti * TT, 2 + (ti + 1) * TT), ct.v(), pv.v(), ALU.mult)
            a = acc[j % 2]
            P.ts("dve", a.v(), CVj.v(0, 2, S + 2), conv_wT[:, j, 2:3], None, ALU.mult)
            P.stt("dve", a.v(), CVj.v(0, 1, S + 1), conv_wT[:, j, 1:2], a.v(), ALU.mult, ALU.add)
            P.stt("dve", a.v(), CVj.v(0, 0, S), conv_wT[:, j, 0:1], a.v(), ALU.mult, ALU.add)
            for ti in range(NT):
                P.tt("dve", Y.tl(j, ti), a[:, ti * TT:(ti + 1) * TT], Bj.tl(0, ti), ALU.mult)
        out_proj(P, C, X, Y, KC, w_out, st)
    P.barrier()


def out_proj(P, C, X, Y, nkc, w_out, st):
    wos = [P.sb([128, nkc, 256], BF16, "wo", st) for _ in range(2)]
    for db in range(D // 256):
        wo = wos[db % 2]
        load_wblock(P, "pool", wo.v(), w_out, 0, nkc * 128, db * 256, 256)
        for ti in range(NT):
            for d2 in range(2):
                dc = db * 2 + d2
                po = P.ps()
                for k in range(nkc):
                    P.mm(po.v(), wo[:, k, d2 * 128:(d2 + 1) * 128], Y.tl(k, ti), start=(k == 0), stop=(k == nkc - 1))
                P.stt("dve", X.tl(dc, ti), X.tl(dc, ti), ALPHA, po.v(), ALU.mult, ALU.add)


CONST_SPECS = {
    "c_ones_inv": ([128, 128], np.float32),
    "c_ident": ([128, 128], np.float32),
    "c_sel": ([NE, NE * 128], np.float32),
    "c_freq": ([128, 1], np.float32),
    "c_ret_mask": ([128, 4, 128], np.float32),
    "c_ret_qd": ([128, 4, 128], np.float32),
    "c_ret_ki": ([128, 4, 128], np.float32),
    "c_b_mask": ([128, 3, 128], np.float32),
    "c_b_ones": ([128, 128], np.float32),
}


def make_consts():
    c = {}
    c["c_ones_inv"] = np.full((128, 128), 1.0 / D, np.float32)
    c["c_ident"] = np.eye(128, dtype=np.float32)
    sel = np.zeros((NE, NE, 128), np.float32)
    for e in range(NE):
        sel[e, e, :] = 1.0
    c["c_sel"] = sel.reshape(NE, NE * 128)
    fr = (10000.0 ** -np.linspace(0.0, 1.0, 128, dtype=np.float32)).astype(np.float64)
    c["c_freq"] = (fr / (2.0 * np.pi)).astype(np.float32).reshape(128, 1)
    mask = np.zeros((128, 4, 128), np.float64)
    qd = np.zeros((128, 4, 128), np.float64)
    ki = np.zeros((128, 4, 128), np.float64)
    idx = np.arange(128)
    for h in range(4):
        g = RET_G[h]
        for j in range(128):
            for i in range(128):
                if (i // 64) == (j // 64):
                    mask[j, h, i] = 1.0 if i >= j else g ** (2 * (j - i))
                elif j < 64 <= i:
                    mask[j, h, i] = 1.0
        qd[:, h, :] = (g ** (idx + 1.0))[None, :]
        ki[:, h, :] = (g ** (-(idx + 1.0)))[None, :] * (256.0 ** -0.5)
    bm = np.zeros((128, 3, 128), np.float32)
    bo = np.zeros((128, 128), np.float32)
    for hh in range(2):
        for a_ in range(64):
            for b_ in range(64):
                ra, cb = hh * 64 + a_, hh * 64 + b_
                bo[ra, cb] = 1.0
                bm[ra, 0, cb] = 1.0 if a_ < b_ else 0.0
                bm[ra, 1, cb] = 1.0 if a_ > b_ else 0.0
                bm[ra, 2, cb] = 1.0 if a_ <= b_ else 0.0
    c["c_b_mask"] = bm
    c["c_b_ones"] = bo
    c["c_ret_mask"] = mask.astype(np.float32)
    c["c_ret_qd"] = qd.astype(np.float32)
    c["c_ret_ki"] = ki.astype(np.float32)
    return c


PARAM_SHAPES = {
    "ln_mix_gT": [128, DEPTH, KC], "ln_mix_bT": [128, DEPTH, KC], "ln_ffn_gT": [128, DEPTH, KC], "ln_ffn_bT": [128, DEPTH, KC],
    "a_w_in": [D, 3 * D], "a_conv_wT": [128, KC, 3], "a_w_out": [D, D],
    "ffn_w_gu": [2, D, 2 * D_FF], "ffn_w_down": [2, D_FF, D],
    "moe_w_router": [2, D, NE], "moe_w_gu": [2, NE, D, 2 * D_FFE], "moe_w_down": [2, NE, D_FFE, D],
    "c_w_in": [D, 2 * D_RNN], "c_prm": [128, 10, 8], "c_w_gaT": [128, 10, 128], "c_w_gxT": [128, 10, 128],
    "b_prm": [128, 8, 11], "b_gn_tok": [128, 8, 128], "b_w1": [D, 64], "b_a1": [D, 64], "b_g1": [D, 160],
    "b_w2": [64, D], "b_a2": [64, D], "b_g2": [160, D], "b_w_r": [D, D], "b_w_k": [D, D], "b_w_v": [D, D], "b_w_o": [D, D],
    "c_w_out": [D_RNN, D], "pos": [1, S], "d_w_in": [D, 6 * D], "d_w_o": [2 * D, D],
}


STAGE_PARAMS = {
    "mixA": ["a_w_in", "a_conv_wT", "a_w_out"],
    "ffn": ["ffn_w_gu", "ffn_w_down"],
    "moe": ["moe_w_router", "moe_w_gu", "moe_w_down"],
    "mixC": ["c_w_in", "c_prm", "c_w_gaT", "c_w_gxT", "c_w_out", "pos"],
    "mixD": ["d_w_in", "d_w_o", "pos"],
    "mixB": ["b_prm", "b_gn_tok", "b_w1", "b_a1", "b_g1", "b_w2", "b_a2", "b_g2", "b_w_r", "b_w_k", "b_w_v", "b_w_o"],
}
ALWAYS_PARAMS = ["ln_mix_gT", "ln_mix_bT", "ln_ffn_gT", "ln_ffn_bT"]


def needed_params(stages):
    need = list(ALWAYS_PARAMS)
    for kind, _ in stages:
        for k in STAGE_PARAMS[kind]:
            if k not in need:
                need.append(k)
    return need


def build(stages):
    nc = bass.Bass("TRN2", target_bir_lowering=False)
    dr = {}
    dr["xT"] = nc.dram_tensor("xT", [D, S], F32, kind="ExternalInput").ap()
    kinds = set(k for k, _ in stages)
    for k in needed_params(stages):
        shp = PARAM_SHAPES[k]
        dr[k] = nc.dram_tensor(k, shp, I32 if k == "pos" else F32, kind="ExternalInput").ap()
    for k, (shp, dt) in CONST_SPECS.items():
        dr[k] = nc.dram_tensor(k, shp, F32, kind="ExternalInput").ap()
    outT = nc.dram_tensor("outT", [D, S], F32, kind="ExternalOutput").ap()

    with ExitStack() as st:
        P = Prog(nc, st)
        C = Ctx()
        C.dr = dr
        P.psb = [P.pst([128, TT], F32, "psb") for _ in range(8)]
        X = P.grid(KC, S, F32, "X")
        Xb = P.grid(KC, S, BF16, "Xb")
        C.ones_inv = P.sb([128, 128], F32, "ones_inv")
        P.dma("sp", C.ones_inv.v(), dr["c_ones_inv"], in_is_dram=True)
        C.ident = P.sb([128, 128], F32, "ident")
        P.dma("sp", C.ident.v(), dr["c_ident"], in_is_dram=True)
        lnp = {}
        for k in ("ln_mix_gT", "ln_mix_bT", "ln_ffn_gT", "ln_ffn_bT"):
            lnp[k] = P.sb([128, DEPTH, KC], F32, k)
            P.dma("sp", lnp[k].v(), dr[k], in_is_dram=True)
        if "mixA" in kinds:
            a_conv = P.sb([128, KC, 3], F32, "a_conv")
            P.dma("sp", a_conv.v(), dr["a_conv_wT"], in_is_dram=True)
        pos_nr = None
        xsrc = dr["xT"].rearrange("(c p) t -> p c t", p=128)
        for c in range(KC):
            for ti in range(NT):
                P.dma("sp", X.tl(c, ti), xsrc[:, c, ti * TT:(ti + 1) * TT], in_is_dram=True)
                P.copy("pool" if (c + ti) % 2 else "dve", Xb.tl(c, ti), X.tl(c, ti))

        for stg in stages:
            kind, li = stg
            if kind == "mixA":
                mixer_a(P, C, X, Xb, dr["a_w_in"], a_conv, dr["a_w_out"])
                layer_norm(P, C, X, X, Xb, lnp["ln_mix_gT"][:, li, :], lnp["ln_mix_bT"][:, li, :])
            elif kind == "mixC":
                mixer_c(P, C, X, Xb, dr)
                layer_norm(P, C, X, X, Xb, lnp["ln_mix_gT"][:, li, :], lnp["ln_mix_bT"][:, li, :])
            elif kind == "mixB":
                mixer_b(P, C, X, Xb, dr)
                layer_norm(P, C, X, X, Xb, lnp["ln_mix_gT"][:, li, :], lnp["ln_mix_bT"][:, li, :])
            elif kind == "mixD":
                mixer_d(P, C, X, Xb, dr)
                layer_norm(P, C, X, X, Xb, lnp["ln_mix_gT"][:, li, :], lnp["ln_mix_bT"][:, li, :])
            elif kind == "moe":
                j = li // 2
                moe(P, C, X, Xb, dr["moe_w_router"][j], dr["moe_w_gu"][j], dr["moe_w_down"][j])
                layer_norm(P, C, X, X, Xb, lnp["ln_ffn_gT"][:, li, :], lnp["ln_ffn_bT"][:, li, :])
            elif kind == "ffn":
                j = li // 2
                ffn(P, C, X, Xb, [(dr["ffn_w_gu"][j], dr["ffn_w_down"][j])], D_FF)
                layer_norm(P, C, X, X, Xb, lnp["ln_ffn_gT"][:, li, :], lnp["ln_ffn_bT"][:, li, :])

        odst = outT.rearrange("(c p) t -> p c t", p=128)
        for c in range(KC):
            for ti in range(NT):
                P.dma("sp", odst[:, c, ti * TT:(ti + 1) * TT], X.tl(c, ti), out_is_dram=True, final=True)
        P.finish("sp")
        P.replay()
    return nc


def colT(v, nchunks):
    return np.ascontiguousarray(np.asarray(v, np.float32).reshape(nchunks, 128).T)


def prep_params(inp):
    p = {}
    for k in ("ln_mix_g", "ln_mix_b", "ln_ffn_g", "ln_ffn_b"):
        a = np.asarray(inp[k], np.float32)
        p[k + "T"] = np.ascontiguousarray(a.reshape(DEPTH, KC, 128).transpose(2, 0, 1))
    p["a_w_in"] = np.ascontiguousarray(inp["a_w_in"], np.float32)
    p["a_conv_wT"] = np.ascontiguousarray(np.asarray(inp["a_conv_w"], np.float32).reshape(3, KC, 128).transpose(2, 1, 0))
    p["a_w_out"] = np.ascontiguousarray(inp["a_w_out"], np.float32)
    p["ffn_w_gu"] = np.ascontiguousarray(inp["ffn_w_gu"], np.float32)
    p["ffn_w_down"] = np.ascontiguousarray(inp["ffn_w_down"], np.float32)
    for k in ("moe_w_router", "moe_w_gu", "moe_w_down", "c_w_in", "c_w_out", "d_w_in", "d_w_o"):
        p[k] = np.ascontiguousarray(inp[k], np.float32)
    for k in ("b_w1", "b_a1", "b_g1", "b_w2", "b_a2", "b_g2", "b_w_r", "b_w_k", "b_w_v", "b_w_o"):
        p[k] = np.ascontiguousarray(inp[k], np.float32)
    bp = np.zeros((128, 8, 11), np.float32)
    bmix = np.asarray(inp["b_mix"], np.float32)
    for j in range(6):
        bp[:, :, j] = colT(bmix[j], 8)
    bp[:, :, 6] = colT(inp["b_w0"], 8)
    bp[:, :, 7] = colT(inp["b_a0"], 8)
    bp[:, :, 8] = colT(inp["b_k_k"], 8)
    bp[:, :, 9] = colT(inp["b_k_a"], 8)
    bp[:, :, 10] = colT(np.asarray(inp["b_r_k"], np.float32).reshape(-1), 8)
    p["b_prm"] = bp
    gg = np.asarray(inp["b_gn_g"], np.float32).reshape(8, 2, 64)
    gb = np.asarray(inp["b_gn_b"], np.float32).reshape(8, 2, 64)
    gt = np.zeros((128, 8, 128), np.float32)
    for hh in range(2):
        gt[hh * 64:(hh + 1) * 64, :, 0:64] = gg[:, hh, :][None, :, :]
        gt[hh * 64:(hh + 1) * 64, :, 64:128] = gb[:, hh, :][None, :, :]
    p["b_gn_tok"] = gt
    cw = np.asarray(inp["c_conv_w"], np.float32)
    prm = np.zeros((128, 10, 8), np.float32)
    for kk in range(4):
        prm[:, :, kk] = colT(cw[kk], 10)
    prm[:, :, 4] = colT(inp["c_conv_b"], 10)
    prm[:, :, 5] = colT(inp["c_b_ga"], 10)
    prm[:, :, 6] = colT(inp["c_b_gx"], 10)
    prm[:, :, 7] = colT(inp["c_lam"], 10)
    p["c_prm"] = prm
    p["c_w_gaT"] = np.ascontiguousarray(np.asarray(inp["c_w_ga"], np.float32).transpose(1, 0, 2))
    p["c_w_gxT"] = np.ascontiguousarray(np.asarray(inp["c_w_gx"], np.float32).transpose(1, 0, 2))
    return p


ALL_STAGES = [("mixA", 0), ("ffn", 0), ("mixB", 1), ("moe", 1), ("mixC", 2), ("ffn", 2), ("mixD", 3), ("moe", 3)]


def run_stages(inp, stages, trace=False):
    nc = build(stages)
    params = prep_params(inp)
    consts = make_consts()
    x = np.asarray(inp["x"], np.float32)
    in_maps = []
    need = needed_params(stages)
    pos = np.asarray(inp["positions"]).astype(np.int32)
    for b in range(N_CORES):
        m = {"xT": np.ascontiguousarray(x[b].T)}
        params["pos"] = np.ascontiguousarray(pos[b].reshape(1, S))
        m.update({k: params[k] for k in need})
        m.update(consts)
        in_maps.append(m)
    res = run_bass_kernel_spmd(nc, in_maps, core_ids=list(range(N_CORES)), trace=trace)
    out = np.stack([np.ascontiguousarray(r["outT"].T) for r in res.results], axis=0)
    return out.astype(np.float32), res


def kernel(**inputs):
    out, _ = run_stages(inputs, ALL_STAGES)
    return out
```

```python
import math
import numpy as np
from contextlib import ExitStack
import concourse.bass as bass
import concourse.mybir as mybir
from concourse.bass_utils import run_bass_kernel_spmd

F32 = mybir.dt.float32
BF16 = mybir.dt.bfloat16
I32 = mybir.dt.int32
ALU = mybir.AluOpType
AF = mybir.ActivationFunctionType
AX = mybir.AxisListType

D = 1024
S = 2048
KC = 8
TT = 512
NT = S // TT
DEPTH = 4
ALPHA = (2.0 * DEPTH) ** 0.25
LN_EPS = 1e-5
D_FF = 2816
D_FFE = 3584
NE = 8
D_RNN = 1280
SEM_M = 30000
N_CORES = 8


class Buf:
    __slots__ = ("name", "lw", "rd")

    def __init__(self, name):
        self.name = name
        self.lw = None
        self.rd = {}


class View:
    __slots__ = ("bufs", "ap")

    def __init__(self, bufs, ap):
        self.bufs = tuple(bufs)
        self.ap = ap

    def __getitem__(self, idx):
        return View(self.bufs, self.ap[idx])

    def re(self, pattern_, **kw):
        return View(self.bufs, self.ap.rearrange(pattern_, **kw))

    def bc(self, shape):
        return View(self.bufs, self.ap.to_broadcast(shape))

    def cast(self, dt):
        return View(self.bufs, self.ap.bitcast(dt))


class Tile:
    def __init__(self, name, t):
        self.buf = Buf(name)
        self.t = t

    def __getitem__(self, idx):
        return View((self.buf,), self.t[idx])

    def v(self):
        return View((self.buf,), self.t[:])


class Grid:
    def __init__(self, name, t, C, T, tile):
        self.t = t
        self.C, self.T, self.tile = C, T, tile
        self.bufs = [[Buf(f"{name}_{c}_{i}") for i in range((T + tile - 1) // tile)] for c in range(C)]

    def v(self, c, t0, t1):
        i0, i1 = t0 // self.tile, (t1 - 1) // self.tile
        return View(self.bufs[c][i0:i1 + 1], self.t[:, c, t0:t1])

    def tl(self, c, i):
        return self.v(c, i * self.tile, min(self.T, (i + 1) * self.tile))

    def full(self, c):
        return self.v(c, 0, self.T)

    def multi(self, c0, c1, t0, t1):
        i0, i1 = t0 // self.tile, (t1 - 1) // self.tile
        bufs = [b for c in range(c0, c1) for b in self.bufs[c][i0:i1 + 1]]
        return View(bufs, self.t[:, c0:c1, t0:t1])


ENGS = ("pe", "act", "dve", "pool", "sp")


class Prog:
    def __init__(self, nc, stack):
        self.nc = nc
        self.st = stack
        self.ops = {e: [] for e in ENGS}
        self.cnt = {e: 0 for e in ENGS}
        self.seen = {e: {} for e in ENGS}
        self.sems = {}
        self.same_sync = {"pe": False, "act": True, "dve": True, "pool": True, "sp": False}
        self.dma_pool = {}
        self.dma_rr = {}
        self.ndma = {"sp": 12, "pool": 12, "act": 8}
        self.uid = 0
        self.ps_rr = 0
        self.out_tokens = []

    def name(self, p):
        self.uid += 1
        return f"{p}_{self.uid}"

    def sem(self, key):
        if key not in self.sems:
            self.sems[key] = self.st.enter_context(self.nc.semaphore(self.name("s_" + "_".join(str(k) for k in key))))
        return self.sems[key]

    def sb(self, shape, dt, name="t", st=None):
        st = st or self.st
        nm = self.name(name)
        t = st.enter_context(self.nc.sbuf_tensor(nm, list(shape), dt))
        return Tile(nm, t)

    def grid(self, C, T, dt, name="g", tile=TT, st=None):
        st = st or self.st
        nm = self.name(name)
        t = st.enter_context(self.nc.sbuf_tensor(nm, [128, C, T], dt))
        return Grid(nm, t, C, T, tile)

    def pst(self, shape, dt, name="ps"):
        nm = self.name(name)
        t = self.st.enter_context(self.nc.psum_tensor(nm, list(shape), dt))
        return Tile(nm, t)

    def _tok_engine(self, eng):
        idx = self.cnt[eng]
        self.cnt[eng] += 1
        key = (eng, idx // SEM_M)
        self.sem(key)
        return (key, idx % SEM_M + 1)

    def _tok_dma(self, eng, waits):
        if eng not in self.dma_pool:
            self.dma_pool[eng] = [[("dma", eng, i), 0] for i in range(self.ndma[eng])]
            self.dma_rr[eng] = 0
        pool = self.dma_pool[eng]
        ent = pool[self.dma_rr[eng] % len(pool)]
        self.dma_rr[eng] += 1
        key, last = ent
        self.sem(key)
        if last > 0 and self.seen[eng].get(key, 0) < last:
            self.seen[eng][key] = last
            waits.append((key, last))
        ent[1] = last + 16
        return (key, last + 16)

    def emit(self, eng, fn, reads=(), writes=(), dma=False):
        deps = {}

        def add(tok):
            k, v = tok
            if deps.get(k, 0) < v:
                deps[k] = v

        rb = [b for v in reads for b in v.bufs]
        wb = [b for v in writes for b in v.bufs]
        for b in rb:
            if b.lw:
                add(b.lw)
        for b in wb:
            if b.lw:
                add(b.lw)
            for k, v in b.rd.items():
                add((k, v))
        waits = []
        for k, v in deps.items():
            if k[0] == eng and not self.same_sync[eng]:
                continue
            if self.seen[eng].get(k, 0) >= v:
                continue
            self.seen[eng][k] = v
            waits.append((k, v))
        tok = self._tok_dma(eng, waits) if dma else self._tok_engine(eng)
        self.ops[eng].append((waits, fn, tok, dma))
        for b in rb:
            if b.rd.get(tok[0], 0) < tok[1]:
                b.rd[tok[0]] = tok[1]
        for b in wb:
            b.lw = tok
            b.rd = {}
        return tok

    def barrier(self):
        toks = []
        for e in ENGS:
            if self.cnt[e] > 0:
                idx = self.cnt[e] - 1
                toks.append(((e, idx // SEM_M), idx % SEM_M + 1))
        for e, pool in self.dma_pool.items():
            for key, last in pool:
                if last > 0:
                    toks.append((key, last))
        for e in ENGS:
            waits = []
            for k, v in toks:
                if k[0] == e and k[0] != "dma" and not self.same_sync[e]:
                    pass
                if self.seen[e].get(k, 0) >= v:
                    continue
                self.seen[e][k] = v
                waits.append((k, v))
            if waits:
                self.ops[e].append((waits, None, None, False))

    def mm(self, out, lhsT, rhs, start=True, stop=True):
        o, l, r = out.ap, lhsT.ap, rhs.ap
        self.emit("pe", lambda e: e.matmul(o, l, r, start=start, stop=stop), reads=(lhsT, rhs), writes=(out,))

    def transpose(self, out, in_, ident):
        o, i, d = out.ap, in_.ap, ident.ap
        self.emit("pe", lambda e: e.transpose(o, i, d), reads=(in_, ident), writes=(out,))

    def tt(self, eng, out, in0, in1, op):
        o, a, b = out.ap, in0.ap, in1.ap
        self.emit(eng, lambda e: e.tensor_tensor(o, a, b, op), reads=(in0, in1), writes=(out,))

    def ts(self, eng, out, in0, s1, s2, op0, op1=None):
        o, a = out.ap, in0.ap
        reads = [in0]
        if isinstance(s1, View):
            reads.append(s1)
            s1 = s1.ap
        if isinstance(s2, View):
            reads.append(s2)
            s2 = s2.ap
        if op1 is None:
            self.emit(eng, lambda e: e.tensor_scalar(o, a, s1, None, op0), reads=reads, writes=(out,))
        else:
            self.emit(eng, lambda e: e.tensor_scalar(o, a, s1, s2, op0, op1), reads=reads, writes=(out,))

    def stt(self, eng, out, in0, scalar, in1, op0, op1):
        o, a, b = out.ap, in0.ap, in1.ap
        reads = [in0, in1]
        if isinstance(scalar, View):
            reads.append(scalar)
            scalar = scalar.ap
        self.emit(eng, lambda e: e.scalar_tensor_tensor(o, a, scalar, b, op0, op1), reads=reads, writes=(out,))

    def copy(self, eng, out, in_):
        o, a = out.ap, in_.ap
        if eng == "act":
            self.emit(eng, lambda e: e.copy(o, a), reads=(in_,), writes=(out,))
        else:
            self.emit(eng, lambda e: e.tensor_copy(o, a), reads=(in_,), writes=(out,))

    def act(self, out, in_, func, bias=None, scale=None, accum_out=None):
        o, a = out.ap, in_.ap
        reads = [in_]
        writes = [out]
        kw = {}
        if bias is not None:
            if isinstance(bias, View):
                reads.append(bias)
                bias = bias.ap
            kw["bias"] = bias
        if scale is not None:
            if isinstance(scale, View):
                reads.append(scale)
                scale = scale.ap
            kw["scale"] = scale
        if accum_out is not None:
            writes.append(accum_out)
            kw["accum_out"] = accum_out.ap
        self.emit("act", lambda e: e.activation(o, a, func, **kw), reads=reads, writes=writes)

    def recip(self, out, in_):
        o, a = out.ap, in_.ap
        self.emit("dve", lambda e: e.reciprocal(o, a), reads=(in_,), writes=(out,))

    def vmax(self, out, in_):
        o, a = out.ap, in_.ap
        self.emit("dve", lambda e: e.max(o, a), reads=(in_,), writes=(out,))

    def memset(self, eng, out, val):
        o = out.ap
        self.emit(eng, lambda e: e.memset(o, val), writes=(out,))

    def scan(self, out, d0, d1, init, op0, op1):
        o, a, b = out.ap, d0.ap, d1.ap
        reads = [d0, d1]
        if isinstance(init, View):
            reads.append(init)
            init = init.ap
        self.emit("dve", lambda e: e.tensor_tensor_scan(o, a, b, init, op0, op1), reads=reads, writes=(out,))

    def dma(self, eng, out, in_, out_is_dram=False, in_is_dram=False, final=False):
        o = out if out_is_dram else out.ap
        a = in_ if in_is_dram else in_.ap
        reads = () if in_is_dram else (in_,)
        writes = () if out_is_dram else (out,)
        tok = self.emit(eng, lambda e: e.dma_start(out=o, in_=a), reads=reads, writes=writes, dma=True)
        if final:
            self.out_tokens.append(tok)
        return tok

    def ps(self):
        b = self.psb[self.ps_rr % len(self.psb)]
        self.ps_rr += 1
        return b

    def finish(self, eng="sp"):
        waits = []
        for k, v in self.out_tokens:
            if self.seen[eng].get(k, 0) < v:
                self.seen[eng][k] = v
                waits.append((k, v))
        self.ops[eng].append((waits, None, None, False))

    def replay(self):
        nc = self.nc
        with nc.Block() as block:
            def run(ename):
                def body(e):
                    for waits, fn, tok, dma in self.ops[ename]:
                        for k, v in waits:
                            e.wait_ge(self.sems[k], v)
                        if fn is not None:
                            ins = fn(e)
                            ins.then_inc(self.sems[tok[0]], 16 if dma else 1)
                return body
            block.tensor(run("pe"))
            block.scalar(run("act"))
            block.vector(run("dve"))
            block.gpsimd(run("pool"))
            block.sync(run("sp"))


class Ctx:
    pass


def load_wblock(P, eng, dst, w_dram, r0, nrows, c0, ncols):
    src = w_dram[r0:r0 + nrows, c0:c0 + ncols].rearrange("(kc p) n -> p kc n", p=128)
    P.dma(eng, dst, src, in_is_dram=True)


def layer_norm(P, C, Z, X, Xb, g_col, b_col):
    st = ExitStack()
    sq = [P.sb([128, TT], F32, "lnsq", st) for _ in range(3)]
    accS = [P.sb([128, TT], F32, "lnS", st) for _ in range(2)]
    accQ = [P.sb([128, TT], F32, "lnQ", st) for _ in range(2)]
    mean = [P.sb([128, TT], F32, "lnmean", st) for _ in range(2)]
    rstd = [P.sb([128, TT], F32, "lnrstd", st) for _ in range(2)]
    t1s = [P.sb([128, TT], F32, "lnt", st) for _ in range(3)]
    t2s = [P.sb([128, TT], F32, "lnt2", st) for _ in range(3)]

    def stats(ti):
        i2 = ti % 2
        S_, Q_ = accS[i2], accQ[i2]
        ps_m = P.ps()
        for c in range(KC):
            P.mm(ps_m.v(), C.ones_inv.v(), Z.tl(c, ti), start=(c == 0), stop=(c == KC - 1))
        for c in range(KC):
            q = sq[c % 3]
            P.act(q.v(), Z.tl(c, ti), AF.Square)
            if c == 0:
                P.copy("dve", Q_.v(), q.v())
            else:
                P.tt("dve", Q_.v(), Q_.v(), q.v(), ALU.add)
        P.copy("act", mean[i2].v(), ps_m.v())
        ps_q = P.ps()
        P.mm(ps_q.v(), C.ones_inv.v(), Q_.v())
        P.tt("dve", S_.v(), mean[i2].v(), mean[i2].v(), ALU.mult)
        P.stt("dve", rstd[i2].v(), ps_q.v(), LN_EPS, S_.v(), ALU.add, ALU.subtract)
        P.act(rstd[i2].v(), rstd[i2].v(), AF.Ln)
        P.act(rstd[i2].v(), rstd[i2].v(), AF.Exp, scale=-0.5)

    def apply(ti):
        i2 = ti % 2
        for c in range(KC):
            t1, t2 = t1s[c % 3], t2s[c % 3]
            P.tt("dve", t1.v(), Z.tl(c, ti), mean[i2].v(), ALU.subtract)
            P.tt("dve", t2.v(), t1.v(), rstd[i2].v(), ALU.mult)
            if c < 4:
                P.act(X.tl(c, ti), t2.v(), AF.Identity, bias=b_col[:, c:c + 1], scale=g_col[:, c:c + 1])
            else:
                P.ts("dve", X.tl(c, ti), t2.v(), g_col[:, c:c + 1], b_col[:, c:c + 1], ALU.mult, ALU.add)
            P.act(Xb.tl(c, ti), t2.v(), AF.Identity, bias=b_col[:, c:c + 1], scale=g_col[:, c:c + 1])

    stats(0)
    for ti in range(NT):
        if ti + 1 < NT:
            stats(ti + 1)
        apply(ti)
    st.close()
    P.barrier()


def ffn(P, C, X, Xb, experts, dff, gates=None):
    for c in range(KC):
        for ti in range(NT):
            if (c + ti) % 2 == 0:
                P.act(X.tl(c, ti), X.tl(c, ti), AF.Copy, scale=ALPHA)
            else:
                P.ts("dve", X.tl(c, ti), X.tl(c, ti), ALPHA, None, ALU.mult)
    FB = 512
    blocks = []
    for e, (wgu, wdn) in enumerate(experts):
        f0 = 0
        while f0 < dff:
            fb = min(FB, dff - f0)
            blocks.append((e, wgu, wdn, f0, fb))
            f0 += fb
    with ExitStack() as st:
        NB = 2
        wg = [P.sb([128, KC, FB], BF16, "wg", st) for _ in range(NB)]
        wu = [P.sb([128, KC, FB], BF16, "wu", st) for _ in range(NB)]
        wd = [P.sb([128, FB // 128, D], BF16, "wd", st) for _ in range(NB)]
        hb = [P.grid(FB // 128, TT, BF16, "hb", TT, st) for _ in range(2)]
        sg = [P.sb([128, TT], F32, "sg", st) for _ in range(2)]
        sg2 = [P.sb([128, TT], F32, "sg2", st) for _ in range(2)]
        scnt = [0]

        def load(bi):
            e, wgu, wdn, f0, fb = blocks[bi]
            nfc = fb // 128
            load_wblock(P, "pool", wg[bi % NB][:, :, 0:fb], wgu, 0, D, f0, fb)
            load_wblock(P, "pool", wu[bi % NB][:, :, 0:fb], wgu, 0, D, dff + f0, fb)
            load_wblock(P, "pool", wd[bi % NB][:, 0:nfc, :], wdn, f0, fb, 0, D)

        items = [(bi, ti) for bi in range(len(blocks)) for ti in range(NT)]

        def gate_up(ii):
            bi, ti = items[ii]
            e, wgu, wdn, f0, fb = blocks[bi]
            nfc = fb // 128
            wgb, wub = wg[bi % NB], wu[bi % NB]
            H = hb[ii % 2]
            for fc in range(nfc):
                pg = P.ps()
                pu = P.ps()
                for k in range(KC):
                    P.mm(pg.v(), wgb[:, k, fc * 128:(fc + 1) * 128], Xb.tl(k, ti), start=(k == 0), stop=(k == KC - 1))
                for k in range(KC):
                    P.mm(pu.v(), wub[:, k, fc * 128:(fc + 1) * 128], Xb.tl(k, ti), start=(k == 0), stop=(k == KC - 1))
                s_ = sg[scnt[0] % 2]
                s2 = sg2[scnt[0] % 2]
                scnt[0] += 1
                P.act(s_.v(), pg.v(), AF.Silu)
                if gates is not None:
                    P.tt("dve", s2.v(), s_.v(), gates(e, ti), ALU.mult)
                    P.tt("dve", H.tl(fc, 0), s2.v(), pu.v(), ALU.mult)
                else:
                    P.tt("dve", H.tl(fc, 0), s_.v(), pu.v(), ALU.mult)

        def down(ii):
            bi, ti = items[ii]
            e, wgu, wdn, f0, fb = blocks[bi]
            nfc = fb // 128
            wdb = wd[bi % NB]
            H = hb[ii % 2]
            for dc in range(KC):
                po = P.ps()
                for fc in range(nfc):
                    P.mm(po.v(), wdb[:, fc, dc * 128:(dc + 1) * 128], H.tl(fc, 0), start=(fc == 0), stop=(fc == nfc - 1))
                P.tt("dve", X.tl(dc, ti), X.tl(dc, ti), po.v(), ALU.add)

        load(0)
        if gates is not None:
            gates(blocks[0][0], 0)
        gate_up(0)
        for ii in range(len(items)):
            bi, ti = items[ii]
            if ti == 0 and bi + 1 < len(blocks):
                load(bi + 1)
            if ii + 1 < len(items):
                gate_up(ii + 1)
            down(ii)
    P.barrier()


def moe(P, C, X, Xb, w_router, w_gu, w_down):
    with ExitStack() as st:
        wr = P.sb([128, KC, NE], F32, "wr", st)
        P.dma("sp", wr.v(), w_router.rearrange("(kc p) e -> p kc e", p=128), in_is_dram=True)
        GT = P.sb([NE, S], F32, "GT", st)
        C.sel = P.sb([NE, NE * 128], F32, "sel", st)
        P.dma("sp", C.sel.v(), C.dr["c_sel"], in_is_dram=True)
        lg = [P.sb([128, NE], F32, "lg", st) for _ in range(2)]
        mx = [P.sb([128, 8], F32, "mx", st) for _ in range(2)]
        sm = [P.sb([128, 4], F32, "sm", st) for _ in range(2)]
        e1 = [P.sb([128, NE], F32, "e1", st) for _ in range(2)]
        mk = [P.sb([128, NE], F32, "mk", st) for _ in range(2)]
        for tt_ in range(S // 128):
            pl = P.ps()
            for k in range(KC):
                P.mm(pl[:, 0:NE], X.v(k, tt_ * 128, (tt_ + 1) * 128), wr[:, k, :], start=(k == 0), stop=(k == KC - 1))
            L, M, Sm, E1, MK = lg[tt_ % 2], mx[tt_ % 2], sm[tt_ % 2], e1[tt_ % 2], mk[tt_ % 2]
            P.copy("dve", L.v(), pl[:, 0:NE])
            P.vmax(M.v(), L.v())
            P.ts("dve", Sm[:, 0:1], M[:, 0:1], -1.0, None, ALU.mult)
            P.act(Sm[:, 1:2], M[:, 1:2], AF.Exp, bias=Sm[:, 0:1])
            P.ts("dve", Sm[:, 2:3], Sm[:, 1:2], 1.0, None, ALU.add)
            P.recip(Sm[:, 2:3], Sm[:, 2:3])
            P.act(E1.v(), L.v(), AF.Exp, bias=Sm[:, 0:1])
            P.ts("dve", MK.v(), L.v(), M[:, 1:2], None, ALU.is_ge)
            P.tt("dve", MK.v(), MK.v(), E1.v(), ALU.mult)
            P.ts("dve", MK.v(), MK.v(), Sm[:, 2:3], None, ALU.mult)
            pt = P.ps()
            P.transpose(pt[0:NE, 0:128], MK.v(), C.ident.v())
            P.copy("act", GT[:, tt_ * 128:(tt_ + 1) * 128], pt[0:NE, 0:128])
        gbc = [P.grid(1, S, F32, "gbc", TT, st) for _ in range(2)]
        state = {"e": -1}

        def gates(e, ti):
            if state["e"] != e:
                state["e"] = e
                for t2 in range(NT):
                    pb = P.ps()
                    P.mm(pb.v(), C.sel[:, e * 128:(e + 1) * 128], GT[:, t2 * TT:(t2 + 1) * TT])
                    P.copy("act", gbc[e % 2].tl(0, t2), pb.v())
            return gbc[e % 2].tl(0, ti)

        ffn(P, C, X, Xb, [(w_gu[e], w_down[e]) for e in range(NE)], D_FFE, gates=gates)


def mixer_b(P, C, X, Xb, dr):
    RT = 256
    NRT = S // RT
    NCH = RT // 64
    Y = Xb
    with ExitStack() as st:
        prm = P.sb([128, 8, 12], F32, "bprm", st)
        P.dma("sp", prm[:, :, 0:11], dr["b_prm"], in_is_dram=True)
        P.ts("dve", prm[:, :, 11:12], prm[:, :, 9:10], -1.0, 1.0, ALU.mult, ALU.add)
        w1 = P.sb([128, KC, 64], BF16, "w1", st)
        a1 = P.sb([128, KC, 64], BF16, "a1", st)
        g1 = P.sb([128, KC, 160], BF16, "g1", st)
        load_wblock(P, "pool", w1.v(), dr["b_w1"], 0, D, 0, 64)
        load_wblock(P, "pool", a1.v(), dr["b_a1"], 0, D, 0, 64)
        load_wblock(P, "pool", g1.v(), dr["b_g1"], 0, D, 0, 160)
        w2 = P.sb([64, D], BF16, "w2", st)
        a2 = P.sb([64, D], BF16, "a2", st)
        g2a = P.sb([128, D], BF16, "g2a", st)
        g2b = P.sb([32, D], BF16, "g2b", st)
        P.dma("pool", w2.v(), dr["b_w2"], in_is_dram=True)
        P.dma("pool", a2.v(), dr["b_a2"], in_is_dram=True)
        P.dma("pool", g2a.v(), dr["b_g2"][0:128, :], in_is_dram=True)
        P.dma("pool", g2b.v(), dr["b_g2"][128:160, :], in_is_dram=True)
        msk = P.sb([128, 3, 128], F32, "bmask", st)
        P.dma("sp", msk.v(), dr["c_b_mask"], in_is_dram=True)
        bones = P.sb([128, 128], F32, "bones", st)
        P.dma("sp", bones.v(), dr["c_b_ones"], in_is_dram=True)
        identb = P.sb([128, 128], BF16, "identb", st)
        P.copy("dve", identb.v(), C.ident.v())
        onec = P.sb([128, 1], BF16, "onec", st)
        P.memset("dve", onec.v(), 1.0)
        cmask = P.sb([128, RT], F32, "cmask", st)
        P.memset("dve", cmask.v(), 1.0)
        P.memset("dve", cmask.v().re("p (c s) -> p c s", s=64)[:, :, 0:1], 0.0)
        H = P.grid(8, 128, F32, "H", 128, st)
        Hb = P.grid(8, 128, BF16, "Hb", 128, st)
        for hp in range(8):
            P.memset("pool", H.tl(hp, 0), 0.0)
            P.memset("pool", Hb.tl(hp, 0), 0.0)
        xx = P.grid(KC, RT, F32, "xx", RT, st)
        xj = [P.grid(KC, RT, BF16, "xj", RT, st) for _ in range(6)]
        twb = P.sb([64, RT], BF16, "twb", st)
        tab = P.sb([64, RT], BF16, "tab", st)
        tgb0 = P.sb([128, RT], BF16, "tgb0", st)
        tgb1 = P.sb([32, RT], BF16, "tgb1", st)
        wrkv = [P.sb([128, KC, 128], BF16, "wrkv", st) for _ in range(3)]

        def F(nm, dt=F32):
            return P.sb([128, RT], dt, nm, st)
        sg, Lc, Lp, EPi, EPp, asig, k_, r_, kk, t1, t2, t3, k2 = (F(n) for n in ("sg", "Lc", "Lp", "EPi", "EPp", "asig", "k_", "r_", "kk", "t1", "t2", "t3", "k2"))
        t4, t5 = Lp, Lc
        EP2 = [F("EP"), F("EP")]
        Gt2 = [F("Gt"), F("Gt")]
        gn2 = [P.sb([128, 128], F32, "gn2", st) for _ in range(2)]
        EXP2 = []
        for bs_ in range(2):
            EXP = {}
            for nm in ("A", "B", "K", "R", "V", "Q"):
                EXP[nm] = P.grid(NCH, 128, BF16, "E" + nm, 128, st)
                for c in range(NCH):
                    P.memset("pool", EXP[nm].tl(c, 0), 0.0)
            EXP2.append(EXP)
        yc4 = P.sb([128, NCH, 64], F32, "yc4", st)
        yq4 = P.sb([128, NCH, 64], F32, "yq4", st)
        st4 = P.sb([128, 2, NCH], F32, "st4", st)
        sb4 = P.sb([128, NCH], F32, "sb4", st)
        NPOOL = 32
        sq_pool = [P.sb([128, 128], BF16, "sq", st) for _ in range(NPOOL)]
        sqi = [0]

        def SQ():
            t = sq_pool[sqi[0] % NPOOL]
            sqi[0] += 1
            return t
        fixed = {nm: [P.sb([128, 128], BF16, nm, st) for _ in range(2 if nm in ("Wb", "Ub") else NCH)] * (2 if nm in ("Wb", "Ub") else 1) for nm in ("VEt", "BEt", "KEt", "AKt", "RBt", "RKt", "Wb", "Ub", "YE")}
        for t in fixed["YE"]:
            P.memset("pool", t.v(), 0.0)
        cc = 0

        def half_expand(eng, E, fn):
            for hh in range(2):
                rows = slice(hh * 64, hh * 64 + 64)
                outv = View([b for c in range(NCH) for b in E.bufs[c]], E.t[rows, :, hh * 64:hh * 64 + 64])
                fn(rows, outv)

        def v3(view, rows):
            return view[rows, :].re("p (c s) -> p c s", s=64)

        pending = None
        for rt in range(NRT):
            t0 = rt * RT

            def shared_gen(rt=rt, t0=t0):
                for kc in range(KC):
                    P.tt("pool", xx.v(kc, 1, RT), X.v(kc, t0, t0 + RT - 1), X.v(kc, t0 + 1, t0 + RT), ALU.subtract)
                    if rt == 0:
                        P.ts("dve", xx.v(kc, 0, 1), X.v(kc, 0, 1), -1.0, None, ALU.mult)
                    else:
                        P.tt("dve", xx.v(kc, 0, 1), X.v(kc, t0 - 1, t0), X.v(kc, t0, t0 + 1), ALU.subtract)
                    yield
                    for j in range(6):
                        P.stt("dve", xj[j].tl(kc, 0), xx.tl(kc, 0), prm[:, kc, j:j + 1], X.v(kc, t0, t0 + RT), ALU.mult, ALU.add)
                        if j % 2 == 1:
                            yield
                pw = P.ps()
                for kc in range(KC):
                    P.mm(pw[0:64, 0:RT], w1[:, kc, :], xj[1].tl(kc, 0), start=(kc == 0), stop=(kc == KC - 1))
                P.act(twb.v(), pw[0:64, 0:RT], AF.Tanh)
                yield
                pa = P.ps()
                for kc in range(KC):
                    P.mm(pa[0:64, 0:RT], a1[:, kc, :], xj[4].tl(kc, 0), start=(kc == 0), stop=(kc == KC - 1))
                P.copy("act", tab.v(), pa[0:64, 0:RT])
                yield
                pg0 = P.ps()
                for kc in range(KC):
                    P.mm(pg0[:, 0:RT], g1[:, kc, 0:128], xj[5].tl(kc, 0), start=(kc == 0), stop=(kc == KC - 1))
                P.act(tgb0.v(), pg0[:, 0:RT], AF.Sigmoid)
                yield
                pg1 = P.ps()
                for kc in range(KC):
                    P.mm(pg1[0:32, 0:RT], g1[:, kc, 128:160], xj[5].tl(kc, 0), start=(kc == 0), stop=(kc == KC - 1))
                P.act(tgb1.v(), pg1[0:32, 0:RT], AF.Sigmoid)
                yield

            def load_w(hp):
                for i3, nm in enumerate(("b_w_r", "b_w_k", "b_w_v")):
                    load_wblock(P, "pool", wrkv[i3].v(), dr[nm], 0, D, hp * 128, 128)

            def pair_prep(hp):
                bs = hp % 2
                cols = slice(hp * 128, (hp + 1) * 128)
                EPb, Gtb = EP2[bs], Gt2[bs]
                EX = {n: EXP2[bs][n] for n in EXP2[bs]}
                P.dma("sp", gn2[bs].v(), dr["b_gn_tok"][:, hp, :], in_is_dram=True)
                if hp == 0:
                    load_w(0)
                pr, pk, pv = P.ps(), P.ps(), P.ps()
                for pp, wt, xi in ((pr, wrkv[0], 0), (pk, wrkv[1], 2), (pv, wrkv[2], 3)):
                    for kc in range(KC):
                        P.mm(pp[:, 0:RT], wt[:, kc, :], xj[xi].tl(kc, 0), start=(kc == 0), stop=(kc == KC - 1))
                P.copy("act", k_.v(), pk[:, 0:RT])
                P.copy("act", r_.v(), pr[:, 0:RT])
                half_expand("act", EX["V"], lambda rows, outv: P.copy("act", outv, v3(pv[:, 0:RT], rows)))
                yield
                if hp < 7:
                    load_w(hp + 1)
                pwl, pal, pgl = P.ps(), P.ps(), P.ps()
                P.mm(pwl[:, 0:RT], w2[:, cols], twb.v())
                P.mm(pal[:, 0:RT], a2[:, cols], tab.v())
                P.mm(pgl[:, 0:RT], g2a[:, cols], tgb0.v(), start=True, stop=False)
                P.mm(pgl[:, 0:RT], g2b[:, cols], tgb1.v(), start=False, stop=True)
                P.act(sg.v(), pwl[:, 0:RT], AF.Sigmoid, bias=prm[:, hp, 6:7])
                P.act(asig.v(), pal[:, 0:RT], AF.Sigmoid, bias=prm[:, hp, 7:8])
                P.copy("act", Gtb.v(), pgl[:, 0:RT])
                yield
                P.ts("dve", sg.v(), sg.v(), -math.exp(-0.5), None, ALU.mult)
                yield
                P.scan(Lc.v(), cmask.v(), sg.v(), 0.0, ALU.mult, ALU.add)
                yield
                P.ts("dve", kk.v(), k_.v(), prm[:, hp, 8:9], None, ALU.mult)
                yield
                P.tt("dve", t1.v(), kk.v(), kk.v(), ALU.mult)
                yield
                pss = P.ps()
                P.mm(pss[:, 0:RT], bones.v(), t1.v())
                P.ts("dve", t1.v(), pss[:, 0:RT], 1e-24, None, ALU.max)
                P.tt("pool", Lp.v(), Lc.v(), sg.v(), ALU.subtract)
                yield
                P.act(EPb.v(), Lc.v(), AF.Exp)
                P.act(EPi.v(), Lc.v(), AF.Exp, scale=-1.0)
                yield
                P.act(EPp.v(), Lp.v(), AF.Exp)
                yield
                P.act(t1.v(), t1.v(), AF.Ln)
                P.ts("dve", t2.v(), asig.v(), prm[:, hp, 9:10], prm[:, hp, 11:12], ALU.mult, ALU.add)
                yield
                P.act(t1.v(), t1.v(), AF.Exp, scale=-0.5)
                P.tt("dve", k2.v(), k_.v(), t2.v(), ALU.mult)
                yield
                P.tt("dve", kk.v(), kk.v(), t1.v(), ALU.mult)
                yield
                P.tt("dve", t2.v(), r_.v(), EPb.v(), ALU.mult)
                yield
                half_expand("act", EX["R"], lambda rows, outv: P.copy("act", outv, v3(t2.v(), rows)))
                P.stt("dve", t1.v(), kk.v(), -1.0, EPp.v(), ALU.mult, ALU.mult)
                yield
                half_expand("pool", EX["A"], lambda rows, outv: P.copy("pool", outv, v3(t1.v(), rows)))
                P.tt("dve", t3.v(), kk.v(), asig.v(), ALU.mult)
                yield
                P.tt("dve", t3.v(), t3.v(), EPi.v(), ALU.mult)
                yield
                half_expand("act", EX["B"], lambda rows, outv: P.copy("act", outv, v3(t3.v(), rows)))
                P.tt("dve", t4.v(), k2.v(), EPi.v(), ALU.mult)
                yield
                half_expand("pool", EX["K"], lambda rows, outv: P.copy("pool", outv, v3(t4.v(), rows)))
                P.stt("dve", t5.v(), r_.v(), prm[:, hp, 10:11], k2.v(), ALU.mult, ALU.mult)
                yield
                half_expand("act", EX["Q"], lambda rows, outv: P.copy("act", outv, v3(t5.v(), rows)))
                yield

            def pair_chunks(hp, t0):
                bs = hp % 2
                EPb, Gtb, gnb = EP2[bs], Gt2[bs], gn2[bs]
                EX = EXP2[bs]
                CH = []
                for c in range(NCH):
                    d_ = {}
                    d_["EA"], d_["EB"], d_["EK"], d_["ER"], d_["EV"], d_["EQ"] = (EX[n].tl(c, 0) for n in ("A", "B", "K", "R", "V", "Q"))
                    for n in ("VEt", "BEt", "KEt", "AKt", "RBt", "RKt", "Wb", "Ub", "YE"):
                        d_[n] = fixed[n][c]
                    CH.append(d_)
                for c in range(NCH):
                    d_ = CH[c]
                    for src, dst in ((d_["EV"], d_["VEt"]), (d_["EB"], d_["BEt"]), (d_["EK"], d_["KEt"])):
                        ptt = P.ps().v().cast(BF16)
                        P.transpose(ptt[:, 0:128], src, identb.v())
                        P.copy("act", dst.v(), ptt[:, 0:128])
                        yield
                for c in range(NCH):
                    d_ = CH[c]
                    Nm, Lm, T, Tt = SQ(), SQ(), SQ(), SQ()
                    p_ = P.ps()
                    P.mm(p_[:, 0:128], d_["EB"], d_["EA"])
                    P.tt("dve", Nm.v(), p_[:, 0:128], msk[:, 0, :], ALU.mult)
                    P.tt("pool", T.v(), Nm.v(), identb.v(), ALU.add)
                    yield
                    p_ = P.ps()
                    P.mm(p_[:, 0:128], d_["EA"], d_["EB"])
                    P.tt("dve", Lm.v(), p_[:, 0:128], msk[:, 1, :], ALU.mult)
                    P.tt("pool", Tt.v(), Lm.v(), identb.v(), ALU.add)
                    d_["N"], d_["L"], d_["T"], d_["Tt"] = Nm, Lm, T, Tt
                    yield
                for c in range(NCH):
                    d_ = CH[c]
                    for (l_, r_op, dst, mi) in ((d_["EK"], d_["EA"], d_["AKt"], 0), (d_["EB"], d_["ER"], d_["RBt"], 2), (d_["EK"], d_["ER"], d_["RKt"], 2)):
                        p_ = P.ps()
                        P.mm(p_[:, 0:128], l_, r_op)
                        P.tt("dve", dst.v(), p_[:, 0:128], msk[:, mi, :], ALU.mult)
                        yield
                for lvl in range(5):
                    last = (lvl == 4)
                    for c in range(NCH):
                        d_ = CH[c]
                        N2 = SQ()
                        p_ = P.ps()
                        P.mm(p_[:, 0:128], d_["L"].v(), d_["N"].v())
                        P.copy("act", N2.v(), p_[:, 0:128])
                        d_["N2"] = N2
                        yield
                        if not last:
                            L2 = SQ()
                            p_ = P.ps()
                            P.mm(p_[:, 0:128], d_["N"].v(), d_["L"].v())
                            P.copy("act", L2.v(), p_[:, 0:128])
                            d_["L2"] = L2
                            yield
                    for c in range(NCH):
                        d_ = CH[c]
                        Tn = SQ()
                        p_ = P.ps()
                        P.mm(p_[:, 0:128], d_["Tt"].v(), d_["N2"].v())
                        P.tt("dve", Tn.v(), p_[:, 0:128], d_["T"].v(), ALU.add)
                        yield
                        if not last:
                            Ttn = SQ()
                            p_ = P.ps()
                            P.mm(p_[:, 0:128], d_["T"].v(), d_["L2"].v())
                            P.tt("dve", Ttn.v(), p_[:, 0:128], d_["Tt"].v(), ALU.add)
                            d_["N"], d_["L"], d_["T"], d_["Tt"] = d_["N2"], d_["L2"], Tn, Ttn
                            yield
                        else:
                            d_["T"] = Tn
                Hbv = Hb.tl(hp, 0)
                Hv = H.tl(hp, 0)
                yield "C"
                pb4 = P.ps()
                for c in range(NCH):
                    P.mm(pb4[:, c:c + 1], CH[c]["EQ"], onec.v())
                P.copy("act", sb4.v(), pb4[:, 0:NCH])
                yield
                for c in range(NCH):
                    d_ = CH[c]
                    p_ = P.ps()
                    P.mm(p_[:, 0:128], d_["EA"], Hbv, start=True, stop=False)
                    P.mm(p_[:, 0:128], d_["AKt"].v(), d_["VEt"].v(), start=False, stop=True)
                    P.copy("act", d_["Wb"].v(), p_[:, 0:128])
                    yield
                    p_ = P.ps()
                    P.mm(p_[:, 0:128], d_["T"].v(), d_["Wb"].v())
                    P.copy("act", d_["Ub"].v(), p_[:, 0:128])
                    yield
                    pst_ = P.ps()
                    P.mm(pst_[:, 0:128], d_["BEt"].v(), d_["Ub"].v(), start=True, stop=False)
                    P.mm(pst_[:, 0:128], d_["KEt"].v(), d_["VEt"].v(), start=False, stop=True)
                    py = P.ps()
                    P.mm(py[:, 0:128], d_["ER"], Hbv, start=True, stop=False)
                    P.mm(py[:, 0:128], d_["RBt"].v(), d_["Ub"].v(), start=False, stop=False)
                    P.mm(py[:, 0:128], d_["RKt"].v(), d_["VEt"].v(), start=False, stop=True)
                    P.tt("dve", Hv, Hv, pst_[:, 0:128], ALU.add)
                    P.copy("act", yc4[0:64, c, :], py[0:64, 0:64])
                    P.copy("act", yc4[64:128, c, :], py[64:128, 64:128])
                    yield
                    P.ts("dve", Hv, Hv, EPb[:, c * 64 + 63:c * 64 + 64], None, ALU.mult)
                    yield
                    P.copy("act", Hbv, Hv)
                    yield
                def red(out, in_):
                    o, a = out.ap, in_.ap
                    P.emit("dve", lambda e: e.reduce_sum(o, a, AX.X), reads=(in_,), writes=(out,))
                red(st4[:, 0, :], yc4.v())
                P.ts("dve", st4[:, 0, :], st4[:, 0, :], -1.0 / 64.0, None, ALU.mult)
                yield
                P.tt("dve", yc4.v(), yc4.v(), st4[:, 0, :].re("p (c o) -> p c o", o=1).bc([128, NCH, 64]), ALU.add)
                yield
                P.tt("pool", yq4.v(), yc4.v(), yc4.v(), ALU.mult)
                yield
                red(st4[:, 1, :], yq4.v())
                P.ts("dve", st4[:, 1, :], st4[:, 1, :], 1.0 / 64.0, 64e-5, ALU.mult, ALU.add)
                yield
                P.act(st4[:, 1, :], st4[:, 1, :], AF.Ln)
                P.act(st4[:, 1, :], st4[:, 1, :], AF.Exp, scale=-0.5)
                yield
                P.tt("dve", yc4.v(), yc4.v(), st4[:, 1, :].re("p (c o) -> p c o", o=1).bc([128, NCH, 64]), ALU.mult)
                yield
                P.tt("dve", yc4.v(), yc4.v(), gnb[:, 0:64].re("p (o v) -> p o v", o=1).bc([128, NCH, 64]), ALU.mult)
                yield
                P.tt("dve", yc4.v(), yc4.v(), gnb[:, 64:128].re("p (o v) -> p o v", o=1).bc([128, NCH, 64]), ALU.add)
                yield
                for c in range(NCH):
                    d_ = CH[c]
                    for hh in range(2):
                        rows = slice(hh * 64, hh * 64 + 64)
                        cb = slice(hh * 64, hh * 64 + 64)
                        P.stt("dve", d_["YE"][rows, cb], d_["VEt"][rows, cb], sb4[rows, c:c + 1], yc4[rows, c, :], ALU.mult, ALU.add)
                    yield
                for c in range(NCH):
                    d_ = CH[c]
                    tok0 = t0 + c * 64
                    ptt = P.ps().v().cast(BF16)
                    P.transpose(ptt[:, 0:128], d_["YE"].v(), identb.v())
                    for hh in range(2):
                        rows = slice(hh * 64, hh * 64 + 64)
                        cb = slice(hh * 64, hh * 64 + 64)
                        P.tt("dve", Y.v(hp, tok0, tok0 + 64)[rows, :], ptt[rows, cb], Gtb[rows, c * 64:(c + 1) * 64], ALU.mult)
                    yield

            def drive(ga, gb, ratio):
                da = ga is None
                db = gb is None
                while not (da and db):
                    if not da:
                        try:
                            next(ga)
                        except StopIteration:
                            da = True
                    for _ in range(ratio):
                        if db:
                            break
                        try:
                            next(gb)
                        except StopIteration:
                            db = True

            def first_gen():
                yield from shared_gen()
                yield from pair_prep(0)

            drive(first_gen(), pending, 2)
            prev = 0
            for hp in range(1, 8):
                drive(pair_prep(hp), pair_chunks(prev, t0), DRIVE_RATIO)
                prev = hp
            pending = pair_chunks(prev, t0)
        drive(None, pending, 1)
        st_out = st.enter_context(ExitStack())
    P.barrier()
    with ExitStack() as st:
        out_proj(P, C, X, Y, KC, dr["b_w_o"], st)
    P.barrier()


def load_posf(P, dr, st):
    posf = P.grid(1, S, F32, "posf", TT, st)
    posi = P.grid(1, S, I32, "posi", TT, st)
    src = dr["pos"].to_broadcast([128, S])
    for ti in range(NT):
        P.dma("sp", posi.tl(0, ti), src[:, ti * TT:(ti + 1) * TT], in_is_dram=True)
        P.copy("dve", posf.tl(0, ti), posi.tl(0, ti))
    return posf


INTERLEAVE = True
DRIVE_RATIO = 4
RET_G = [1.0 - 2.0 ** (-5.0 - h) for h in range(4)]


def mixer_d(P, C, X, Xb, dr):
    w_in, w_o = dr["d_w_in"], dr["d_w_o"]
    with ExitStack() as st:
        cs = P.grid(2, S, F32, "cossin", TT, st)
        with ExitStack() as st2:
            posf = load_posf(P, dr, st2)
            freq = P.sb([128, 1], F32, "freq", st2)
            P.dma("sp", freq.v(), dr["c_freq"], in_is_dram=True)
            a1 = [P.sb([128, TT], F32, "ang", st2) for _ in range(2)]
            ai = [P.sb([128, TT], I32, "angi", st2) for _ in range(2)]
            af = [P.sb([128, TT], F32, "angf", st2) for _ in range(2)]
            am = [P.sb([128, TT], F32, "angm", st2) for _ in range(2)]
            for ti in range(NT):
                for which, shift in ((1, 0.0), (0, 0.25)):
                    a, n_i, n_f, m_ = a1[which], ai[which], af[which], am[which]
                    P.ts("dve", a.v(), posf.tl(0, ti), freq[:, 0:1], shift, ALU.mult, ALU.add)
                    P.copy("dve", n_i.v(), a.v())
                    P.copy("dve", n_f.v(), n_i.v())
                    P.tt("dve", a.v(), a.v(), n_f.v(), ALU.subtract)
                    P.ts("dve", m_.v(), a.v(), 0.5, None, ALU.is_ge)
                    P.tt("dve", a.v(), a.v(), m_.v(), ALU.subtract)
                    P.ts("dve", m_.v(), a.v(), -0.5, None, ALU.is_lt)
                    P.tt("dve", a.v(), a.v(), m_.v(), ALU.add)
                    P.act(cs.tl(which, ti), a.v(), AF.Sin, scale=2.0 * math.pi)
        P.barrier()
        cm = P.sb([128, 4, 128], F32, "rmask", st)
        qdc = P.sb([128, 4, 128], F32, "rqd", st)
        kic = P.sb([128, 4, 128], F32, "rki", st)
        P.dma("sp", cm.v(), dr["c_ret_mask"], in_is_dram=True)
        P.dma("sp", qdc.v(), dr["c_ret_qd"], in_is_dram=True)
        P.dma("sp", kic.v(), dr["c_ret_ki"], in_is_dram=True)
        identb = P.sb([128, 128], BF16, "identb", st)
        P.copy("dve", identb.v(), C.ident.v())
        QD = P.grid(2, S, BF16, "QD", TT, st)
        KI = P.grid(2, S, BF16, "KI", TT, st)
        wqk = [P.sb([128, KC, 256], BF16, "wqk", st) for _ in range(2)]
        wv = P.sb([128, KC, 512], BF16, "wv", st)
        wg = P.sb([128, KC, 512], BF16, "wg", st)
        wo = P.sb([128, 4, D], BF16, "wo_d", st)
        YG = [P.grid(4, TT, BF16, "YG", TT, st) for _ in range(2)]
        Sst = P.sb([128, 2, 512], F32, "Sst", st)
        Sbf = P.sb([128, 2, 512], BF16, "Sbf", st)
        rt = [P.sb([128, TT], F32, "rt", st) for _ in range(4)]
        STb = [P.sb([128, 128], BF16, "STb", st) for _ in range(2)]
        KIt = [P.sb([128, 256], BF16, "KIt", st) for _ in range(2)]
        Vb = [P.sb([128, 512], BF16, "Vb", st) for _ in range(2)]
        sgl = [P.sb([128, 512], F32, "sgl", st) for _ in range(3)]
        yn = [P.sb([128, 512], F32, "yn", st) for _ in range(2)]
        ygb = [P.sb([128, 512], BF16, "ygb", st) for _ in range(2)]
        ssq = [P.sb([128, 2], F32, "ssq", st) for _ in range(2)]
        junk = P.sb([128, 512], F32, "junk", st)
        ygc = 0
        for h in range(4):
            g128 = RET_G[h] ** 128
            for qk in range(2):
                load_wblock(P, "pool", wqk[qk].v(), w_in, 0, D, qk * D + h * 256, 256)
            load_wblock(P, "pool", wv.v(), w_in, 0, D, 2 * D + h * 512, 512)
            load_wblock(P, "pool", wg.v(), w_in, 0, D, 4 * D + h * 512, 512)
            load_wblock(P, "pool", wo.v(), w_o, h * 512, 512, 0, D)
            for qk in range(2):
                wb = wqk[qk]
                dst = QD if qk == 0 else KI
                fac = qdc if qk == 0 else kic
                for ti in range(NT):
                    p1, p2 = P.ps(), P.ps()
                    for c, pp in enumerate((p1, p2)):
                        for k in range(KC):
                            P.mm(pp.v(), wb[:, k, c * 128:(c + 1) * 128], Xb.tl(k, ti), start=(k == 0), stop=(k == KC - 1))
                    cos, sin = cs.tl(0, ti), cs.tl(1, ti)
                    P.tt("dve", rt[0].v(), cos, p1.v(), ALU.mult)
                    P.tt("dve", rt[1].v(), sin, p2.v(), ALU.mult)
                    P.tt("pool", rt[0].v(), rt[0].v(), rt[1].v(), ALU.subtract)
                    P.tt("dve", rt[2].v(), sin, p1.v(), ALU.mult)
                    P.tt("dve", rt[3].v(), cos, p2.v(), ALU.mult)
                    P.tt("pool", rt[2].v(), rt[2].v(), rt[3].v(), ALU.add)
                    fb = fac[:, h:h + 1, :].bc([128, 4, 128])
                    P.tt("pool", dst.tl(0, ti).re("p (a b) -> p a b", b=128), rt[0].v().re("p (a b) -> p a b", b=128), fb, ALU.mult)
                    P.tt("dve", dst.tl(1, ti).re("p (a b) -> p a b", b=128), rt[2].v().re("p (a b) -> p a b", b=128), fb, ALU.mult)
            P.memset("pool", Sst.v(), 0.0)
            P.memset("pool", Sbf.v(), 0.0)
            NM = S // 128

            def stage_a(m):
                t0 = m * 128
                i2 = m % 2
                pss = P.ps()
                for dc in range(2):
                    P.mm(pss[:, 0:128], KI.v(dc, t0, t0 + 128), QD.v(dc, t0, t0 + 128), start=(dc == 0), stop=(dc == 1))
                P.tt("dve", STb[i2].v(), pss[:, 0:128], cm[:, h, :], ALU.mult)
                pv, pg = P.ps(), P.ps()
                for k in range(KC):
                    P.mm(pv.v(), Xb.v(k, t0, t0 + 128), wv[:, k, :], start=(k == 0), stop=(k == KC - 1))
                for k in range(KC):
                    P.mm(pg.v(), Xb.v(k, t0, t0 + 128), wg[:, k, :], start=(k == 0), stop=(k == KC - 1))
                P.copy("act", Vb[i2].v(), pv.v())
                P.act(sgl[m % 3].v(), pg.v(), AF.Silu)
                for dc in range(2):
                    ptt = P.ps()
                    pv_bf = ptt.v().cast(BF16)
                    P.transpose(pv_bf[:, 0:128], KI.v(dc, t0, t0 + 128), identb.v())
                    P.copy("dve", KIt[i2][:, dc * 128:(dc + 1) * 128], pv_bf[:, 0:128])

            def stage_b(m):
                t0 = m * 128
                i2 = m % 2
                po = P.ps()
                P.mm(po.v(), STb[i2].v(), Vb[i2].v(), start=True, stop=False)
                for dc in range(2):
                    P.mm(po.v(), QD.v(dc, t0, t0 + 128), Sbf[:, dc, :], start=False, stop=(dc == 1))
                for dc in range(2):
                    pk = P.ps()
                    P.mm(pk.v(), KIt[i2][:, dc * 128:(dc + 1) * 128], Vb[i2].v())
                    P.tt("dve", Sst[:, dc, :], Sst[:, dc, :], pk.v(), ALU.add)
                P.act(Sst.v(), Sst.v(), AF.Copy, scale=g128)
                P.copy("act", Sbf.v(), Sst.v())
                P.copy("act", yn[i2].v(), po.v())

            def stage_c(m):
                nonlocal ygc
                i2 = m % 2
                ti = m // 4
                Y = YG[ygc % 2]
                P.memset("pool", ssq[i2].v(), 0.0)
                P.act(junk.v(), yn[i2].v(), AF.Square, accum_out=ssq[i2][:, 0:1])
                P.ts("dve", ssq[i2][:, 1:2], ssq[i2][:, 0:1], 1.0 / 512.0, 1e-6, ALU.mult, ALU.add)
                P.act(ssq[i2][:, 1:2], ssq[i2][:, 1:2], AF.Sqrt)
                P.recip(ssq[i2][:, 1:2], ssq[i2][:, 1:2])
                P.stt("dve", ygb[i2].v(), yn[i2].v(), ssq[i2][:, 1:2], sgl[m % 3].v(), ALU.mult, ALU.mult)
                for vc in range(4):
                    ptt = P.ps()
                    pv_bf = ptt.v().cast(BF16)
                    P.transpose(pv_bf[:, 0:128], ygb[i2][:, vc * 128:(vc + 1) * 128], identb.v())
                    P.copy("act" if vc % 2 else "dve", Y.v(vc, (m % 4) * 128, (m % 4 + 1) * 128), pv_bf[:, 0:128])
                if m % 4 == 3:
                    for dc in range(KC):
                        pq = P.ps()
                        for vc in range(4):
                            P.mm(pq.v(), wo[:, vc, dc * 128:(dc + 1) * 128], Y.tl(vc, 0), start=(vc == 0), stop=(vc == 3))
                        if h == 0:
                            P.stt("dve", X.tl(dc, ti), X.tl(dc, ti), ALPHA, pq.v(), ALU.mult, ALU.add)
                        else:
                            P.tt("dve", X.tl(dc, ti), X.tl(dc, ti), pq.v(), ALU.add)
                    ygc += 1

            stage_a(0)
            for m in range(NM):
                if m + 1 < NM:
                    stage_a(m + 1)
                stage_b(m)
                if m > 0:
                    stage_c(m - 1)
            stage_c(NM - 1)
    P.barrier()


def mixer_c(P, C, X, Xb, dr):
    NB = D_RNN // 128
    with ExitStack() as st:
        Y = P.grid(NB, S, BF16, "yc", TT, st)
        st_out = st
        st = st.enter_context(ExitStack())
        prm = P.sb([128, NB, 12], F32, "cprm", st)
        P.dma("sp", prm[:, :, 0:8], dr["c_prm"], in_is_dram=True)
        P.act(prm[:, :, 9:10], prm[:, :, 7:8], AF.Exp, scale=-1.0)
        P.ts("dve", prm[:, :, 9:10], prm[:, :, 9:10], 1.0, None, ALU.add)
        P.act(prm[:, :, 10:11], prm[:, :, 9:10], AF.Ln)
        P.ts("dve", prm[:, :, 8:9], prm[:, :, 10:11], -8.0, None, ALU.mult)
        wga = P.sb([128, NB, 128], BF16, "wga", st)
        wgx = P.sb([128, NB, 128], BF16, "wgx", st)
        P.dma("pool", wga.v(), dr["c_w_gaT"], in_is_dram=True)
        P.dma("pool", wgx.v(), dr["c_w_gxT"], in_is_dram=True)
        pos_nr = P.grid(1, S, BF16, "pos_nr", TT, st)
        with ExitStack() as st2:
            posf = load_posf(P, dr, st2)
            for ti in range(NT):
                P.ts("dve", pos_nr.tl(0, ti), posf.tl(0, ti), 0.0, None, ALU.not_equal)
        P.barrier()
        wblk = [P.sb([128, KC, 256], BF16, "wc", st) for _ in range(2)]
        Upad = [P.grid(1, S + 3, F32, "upad", S + 3, st) for _ in range(2)]
        P.ts("dve", prm[:, :, 9:10], prm[:, :, 5:6], 0.5, None, ALU.mult)
        P.ts("dve", prm[:, :, 10:11], prm[:, :, 6:7], 0.5, None, ALU.mult)
        P.ts("dve", prm[:, :, 11:12], prm[:, :, 8:9], 0.5, None, ALU.mult)
        sets = []
        NSET = 3
        for _ in range(NSET):
            d_ = {}
            for nm in ("A", "I", "GG", "uc", "M", "Ht"):
                d_[nm] = P.sb([128, TT], F32, nm, st)
            sets.append(d_)
        w_in = dr["c_w_in"]

        def tile_gen(g, ti, cnt):
            B_ = sets[cnt % NSET]
            Bp = sets[(cnt - 1) % NSET]
            A, I, GG, uc, M, Ht = B_["A"], B_["I"], B_["GG"], B_["uc"], B_["M"], B_["Ht"]
            ucbv = M.v().cast(BF16)[:, 0:TT]
            wb = wblk[g % 2]
            Up = Upad[g % 2]
            if ti == 0:
                load_wblock(P, "pool", wb[:, :, 0:128], w_in, 0, D, g * 128, 128)
                load_wblock(P, "pool", wb[:, :, 128:256], w_in, 0, D, D_RNN + g * 128, 128)
                P.memset("pool", Up.v(0, 0, 3), 0.0)
            t0 = ti * TT
            pg, pu = P.ps(), P.ps()
            for k in range(KC):
                P.mm(pg.v(), wb[:, k, 0:128], Xb.tl(k, ti), start=(k == 0), stop=(k == KC - 1))
            for k in range(KC):
                P.mm(pu.v(), wb[:, k, 128:256], Xb.tl(k, ti), start=(k == 0), stop=(k == KC - 1))
            P.copy("act", Up.v(0, 3 + t0, 3 + t0 + TT), pu.v())
            P.act(A.v(), pg.v(), AF.Square)
            P.copy("act", I.v(), pg.v())
            yield
            P.ts("dve", A.v(), A.v(), 0.044715, 1.0, ALU.mult, ALU.add)
            yield
            P.tt("dve", A.v(), A.v(), I.v(), ALU.mult)
            yield
            P.act(A.v(), A.v(), AF.Tanh, scale=0.7978845608028654)
            yield
            P.stt("dve", GG.v(), A.v(), 1.0, I.v(), ALU.add, ALU.mult)
            yield
            P.ts("dve", uc.v(), Up.v(0, 3 + t0, 3 + t0 + TT), prm[:, g, 3:4], prm[:, g, 4:5], ALU.mult, ALU.add)
            yield
            for kk in range(3):
                P.stt("dve", uc.v(), Up.v(0, kk + t0, kk + t0 + TT), prm[:, g, kk:kk + 1], uc.v(), ALU.mult, ALU.add)
                yield
            P.copy("act", ucbv, uc.v())
            yield
            pr, pi = P.ps(), P.ps()
            P.mm(pr.v(), wga[:, g, :], ucbv)
            P.mm(pi.v(), wgx[:, g, :], ucbv)
            P.act(A.v(), pr.v(), AF.Tanh, bias=prm[:, g, 9:10], scale=0.5)
            P.act(I.v(), pi.v(), AF.Tanh, bias=prm[:, g, 10:11], scale=0.5)
            yield
            P.act(A.v(), A.v(), AF.Exp, bias=prm[:, g, 11:12], scale=prm[:, g, 11:12])
            yield
            P.tt("pool", A.v(), A.v(), pos_nr.tl(0, ti), ALU.mult)
            yield
            P.tt("dve", M.v(), A.v(), A.v(), ALU.mult)
            yield
            P.ts("dve", M.v(), M.v(), -1.0, 1.0, ALU.mult, ALU.add)
            yield
            P.act(M.v(), M.v(), AF.Sqrt)
            yield
            P.stt("dve", M.v(), I.v(), 1.0, M.v(), ALU.add, ALU.mult)
            yield
            P.stt("dve", M.v(), M.v(), 0.5, uc.v(), ALU.mult, ALU.mult)
            yield
            if ti == 0:
                P.scan(Ht.v(), A.v(), M.v(), 0.0, ALU.mult, ALU.add)
            else:
                P.scan(Ht.v(), A.v(), M.v(), Bp["Ht"][:, TT - 1:TT], ALU.mult, ALU.add)
            yield
            P.stt("dve", Y.tl(g, ti), GG.v(), 0.5, Ht.v(), ALU.mult, ALU.mult)
            yield

        tiles = [(g, ti) for g in range(NB) for ti in range(NT)]
        active = []
        nxt = 0
        while nxt < len(tiles) or active:
            while len(active) < NSET and nxt < len(tiles):
                g_, ti_ = tiles[nxt]
                active.append(tile_gen(g_, ti_, nxt))
                nxt += 1
            for gen in list(active):
                try:
                    next(gen)
                except StopIteration:
                    active.remove(gen)
        st.close()
        P.barrier()
        out_proj(P, C, X, Y, NB, dr["c_w_out"], st_out)
    P.barrier()


def mixer_a(P, C, X, Xb, w_in, conv_wT, w_out):
    with ExitStack() as st:
        Y = P.grid(KC, S, BF16, "ya", TT, st)
        wblk = [P.sb([128, KC, 384], BF16, "wa", st) for _ in range(2)]
        Bt = [P.grid(1, S, F32, "ab", TT, st) for _ in range(2)]
        CV = [P.grid(1, S + 2, F32, "acv", S + 2, st) for _ in range(2)]
        Ct = [P.sb([128, TT], F32, "act_c", st) for _ in range(2)]
        acc = [P.sb([128, S], F32, "aacc", st) for _ in range(2)]
        for j in range(KC):
            wb = wblk[j % 2]
            for q in range(3):
                load_wblock(P, "pool", wb[:, :, q * 128:(q + 1) * 128], w_in, 0, D, q * D + j * 128, 128)
            Bj, CVj = Bt[j % 2], CV[j % 2]
            P.memset("pool", CVj.v(0, 0, 2), 0.0)
            for ti in range(NT):
                pb, pc, pv = P.ps(), P.ps(), P.ps()
                for q, pp in enumerate((pb, pc, pv)):
                    for k in range(KC):
                        P.mm(pp.v(), wb[:, k, q * 128:(q + 1) * 128], Xb.tl(k, ti), start=(k == 0), stop=(k == KC - 1))
                P.copy("act", Bj.tl(0, ti), pb.v())
                ct = Ct[ti % 2]
                P.copy("act", ct.v(), pc.v())
                P.tt("dve", CVj.v(0, 2 + ti * TT, 2 + (ti + 1) * TT), ct.v(), pv.v(), ALU.mult)
            a = acc[j % 2]
            P.ts("dve", a.v(), CVj.v(0, 2, S + 2), conv_wT[:, j, 2:3], None, ALU.mult)
            P.stt("dve", a.v(), CVj.v(0, 1, S + 1), conv_wT[:, j, 1:2], a.v(), ALU.mult, ALU.add)
            P.stt("dve", a.v(), CVj.v(0, 0, S), conv_wT[:, j, 0:1], a.v(), ALU.mult, ALU.add)
            for ti in range(NT):
                P.tt("dve", Y.tl(j, ti), a[:, ti * TT:(ti + 1) * TT], Bj.tl(0, ti), ALU.mult)
        out_proj(P, C, X, Y, KC, w_out, st)
    P.barrier()


def out_proj(P, C, X, Y, nkc, w_out, st):
    wos = [P.sb([128, nkc, 256], BF16, "wo", st) for _ in range(2)]
    for db in range(D // 256):
        wo = wos[db % 2]
        load_wblock(P, "pool", wo.v(), w_out, 0, nkc * 128, db * 256, 256)
        for ti in range(NT):
            for d2 in range(2):
                dc = db * 2 + d2
                po = P.ps()
                for k in range(nkc):
                    P.mm(po.v(), wo[:, k, d2 * 128:(d2 + 1) * 128], Y.tl(k, ti), start=(k == 0), stop=(k == nkc - 1))
                P.stt("dve", X.tl(dc, ti), X.tl(dc, ti), ALPHA, po.v(), ALU.mult, ALU.add)


CONST_SPECS = {
    "c_ones_inv": ([128, 128], np.float32),
    "c_ident": ([128, 128], np.float32),
    "c_sel": ([NE, NE * 128], np.float32),
    "c_freq": ([128, 1], np.float32),
    "c_ret_mask": ([128, 4, 128], np.float32),
    "c_ret_qd": ([128, 4, 128], np.float32),
    "c_ret_ki": ([128, 4, 128], np.float32),
    "c_b_mask": ([128, 3, 128], np.float32),
    "c_b_ones": ([128, 128], np.float32),
}


def make_consts():
    c = {}
    c["c_ones_inv"] = np.full((128, 128), 1.0 / D, np.float32)
    c["c_ident"] = np.eye(128, dtype=np.float32)
    sel = np.zeros((NE, NE, 128), np.float32)
    for e in range(NE):
        sel[e, e, :] = 1.0
    c["c_sel"] = sel.reshape(NE, NE * 128)
    fr = (10000.0 ** -np.linspace(0.0, 1.0, 128, dtype=np.float32)).astype(np.float64)
    c["c_freq"] = (fr / (2.0 * np.pi)).astype(np.float32).reshape(128, 1)
    mask = np.zeros((128, 4, 128), np.float64)
    qd = np.zeros((128, 4, 128), np.float64)
    ki = np.zeros((128, 4, 128), np.float64)
    idx = np.arange(128)
    for h in range(4):
        g = RET_G[h]
        for j in range(128):
            for i in range(128):
                if (i // 64) == (j // 64):
                    mask[j, h, i] = 1.0 if i >= j else g ** (2 * (j - i))
                elif j < 64 <= i:
                    mask[j, h, i] = 1.0
        qd[:, h, :] = (g ** (idx + 1.0))[None, :]
        ki[:, h, :] = (g ** (-(idx + 1.0)))[None, :] * (256.0 ** -0.5)
    bm = np.zeros((128, 3, 128), np.float32)
    bo = np.zeros((128, 128), np.float32)
    for hh in range(2):
        for a_ in range(64):
            for b_ in range(64):
                ra, cb = hh * 64 + a_, hh * 64 + b_
                bo[ra, cb] = 1.0
                bm[ra, 0, cb] = 1.0 if a_ < b_ else 0.0
                bm[ra, 1, cb] = 1.0 if a_ > b_ else 0.0
                bm[ra, 2, cb] = 1.0 if a_ <= b_ else 0.0
    c["c_b_mask"] = bm
    c["c_b_ones"] = bo
    c["c_ret_mask"] = mask.astype(np.float32)
    c["c_ret_qd"] = qd.astype(np.float32)
    c["c_ret_ki"] = ki.astype(np.float32)
    return c


PARAM_SHAPES = {
    "ln_mix_gT": [128, DEPTH, KC], "ln_mix_bT": [128, DEPTH, KC], "ln_ffn_gT": [128, DEPTH, KC], "ln_ffn_bT": [128, DEPTH, KC],
    "a_w_in": [D, 3 * D], "a_conv_wT": [128, KC, 3], "a_w_out": [D, D],
    "ffn_w_gu": [2, D, 2 * D_FF], "ffn_w_down": [2, D_FF, D],
    "moe_w_router": [2, D, NE], "moe_w_gu": [2, NE, D, 2 * D_FFE], "moe_w_down": [2, NE, D_FFE, D],
    "c_w_in": [D, 2 * D_RNN], "c_prm": [128, 10, 8], "c_w_gaT": [128, 10, 128], "c_w_gxT": [128, 10, 128],
    "b_prm": [128, 8, 11], "b_gn_tok": [128, 8, 128], "b_w1": [D, 64], "b_a1": [D, 64], "b_g1": [D, 160],
    "b_w2": [64, D], "b_a2": [64, D], "b_g2": [160, D], "b_w_r": [D, D], "b_w_k": [D, D], "b_w_v": [D, D], "b_w_o": [D, D],
    "c_w_out": [D_RNN, D], "pos": [1, S], "d_w_in": [D, 6 * D], "d_w_o": [2 * D, D],
}


STAGE_PARAMS = {
    "mixA": ["a_w_in", "a_conv_wT", "a_w_out"],
    "ffn": ["ffn_w_gu", "ffn_w_down"],
    "moe": ["moe_w_router", "moe_w_gu", "moe_w_down"],
    "mixC": ["c_w_in", "c_prm", "c_w_gaT", "c_w_gxT", "c_w_out", "pos"],
    "mixD": ["d_w_in", "d_w_o", "pos"],
    "mixB": ["b_prm", "b_gn_tok", "b_w1", "b_a1", "b_g1", "b_w2", "b_a2", "b_g2", "b_w_r", "b_w_k", "b_w_v", "b_w_o"],
}
ALWAYS_PARAMS = ["ln_mix_gT", "ln_mix_bT", "ln_ffn_gT", "ln_ffn_bT"]


def needed_params(stages):
    need = list(ALWAYS_PARAMS)
    for kind, _ in stages:
        for k in STAGE_PARAMS[kind]:
            if k not in need:
                need.append(k)
    return need


def build(stages):
    nc = bass.Bass("TRN2", target_bir_lowering=False)
    dr = {}
    dr["xT"] = nc.dram_tensor("xT", [D, S], F32, kind="ExternalInput").ap()
    kinds = set(k for k, _ in stages)
    for k in needed_params(stages):
        shp = PARAM_SHAPES[k]
        dr[k] = nc.dram_tensor(k, shp, I32 if k == "pos" else F32, kind="ExternalInput").ap()
    for k, (shp, dt) in CONST_SPECS.items():
        dr[k] = nc.dram_tensor(k, shp, F32, kind="ExternalInput").ap()
    outT = nc.dram_tensor("outT", [D, S], F32, kind="ExternalOutput").ap()

    with ExitStack() as st:
        P = Prog(nc, st)
        C = Ctx()
        C.dr = dr
        P.psb = [P.pst([128, TT], F32, "psb") for _ in range(8)]
        X = P.grid(KC, S, F32, "X")
        Xb = P.grid(KC, S, BF16, "Xb")
        C.ones_inv = P.sb([128, 128], F32, "ones_inv")
        P.dma("sp", C.ones_inv.v(), dr["c_ones_inv"], in_is_dram=True)
        C.ident = P.sb([128, 128], F32, "ident")
        P.dma("sp", C.ident.v(), dr["c_ident"], in_is_dram=True)
        lnp = {}
        for k in ("ln_mix_gT", "ln_mix_bT", "ln_ffn_gT", "ln_ffn_bT"):
            lnp[k] = P.sb([128, DEPTH, KC], F32, k)
            P.dma("sp", lnp[k].v(), dr[k], in_is_dram=True)
        if "mixA" in kinds:
            a_conv = P.sb([128, KC, 3], F32, "a_conv")
            P.dma("sp", a_conv.v(), dr["a_conv_wT"], in_is_dram=True)
        pos_nr = None
        xsrc = dr["xT"].rearrange("(c p) t -> p c t", p=128)
        for c in range(KC):
            for ti in range(NT):
                P.dma("sp", X.tl(c, ti), xsrc[:, c, ti * TT:(ti + 1) * TT], in_is_dram=True)
                P.copy("pool" if (c + ti) % 2 else "dve", Xb.tl(c, ti), X.tl(c, ti))

        for stg in stages:
            kind, li = stg
            if kind == "mixA":
                mixer_a(P, C, X, Xb, dr["a_w_in"], a_conv, dr["a_w_out"])
                layer_norm(P, C, X, X, Xb, lnp["ln_mix_gT"][:, li, :], lnp["ln_mix_bT"][:, li, :])
            elif kind == "mixC":
                mixer_c(P, C, X, Xb, dr)
                layer_norm(P, C, X, X, Xb, lnp["ln_mix_gT"][:, li, :], lnp["ln_mix_bT"][:, li, :])
            elif kind == "mixB":
                mixer_b(P, C, X, Xb, dr)
                layer_norm(P, C, X, X, Xb, lnp["ln_mix_gT"][:, li, :], lnp["ln_mix_bT"][:, li, :])
            elif kind == "mixD":
                mixer_d(P, C, X, Xb, dr)
                layer_norm(P, C, X, X, Xb, lnp["ln_mix_gT"][:, li, :], lnp["ln_mix_bT"][:, li, :])
            elif kind == "moe":
                j = li // 2
                moe(P, C, X, Xb, dr["moe_w_router"][j], dr["moe_w_gu"][j], dr["moe_w_down"][j])
                layer_norm(P, C, X, X, Xb, lnp["ln_ffn_gT"][:, li, :], lnp["ln_ffn_bT"][:, li, :])
            elif kind == "ffn":
                j = li // 2
                ffn(P, C, X, Xb, [(dr["ffn_w_gu"][j], dr["ffn_w_down"][j])], D_FF)
                layer_norm(P, C, X, X, Xb, lnp["ln_ffn_gT"][:, li, :], lnp["ln_ffn_bT"][:, li, :])

        odst = outT.rearrange("(c p) t -> p c t", p=128)
        for c in range(KC):
            for ti in range(NT):
                P.dma("sp", odst[:, c, ti * TT:(ti + 1) * TT], X.tl(c, ti), out_is_dram=True, final=True)
        P.finish("sp")
        P.replay()
    return nc


def colT(v, nchunks):
    return np.ascontiguousarray(np.asarray(v, np.float32).reshape(nchunks, 128).T)


def prep_params(inp):
    p = {}
    for k in ("ln_mix_g", "ln_mix_b", "ln_ffn_g", "ln_ffn_b"):
        a = np.asarray(inp[k], np.float32)
        p[k + "T"] = np.ascontiguousarray(a.reshape(DEPTH, KC, 128).transpose(2, 0, 1))
    p["a_w_in"] = np.ascontiguousarray(inp["a_w_in"], np.float32)
    p["a_conv_wT"] = np.ascontiguousarray(np.asarray(inp["a_conv_w"], np.float32).reshape(3, KC, 128).transpose(2, 1, 0))
    p["a_w_out"] = np.ascontiguousarray(inp["a_w_out"], np.float32)
    p["ffn_w_gu"] = np.ascontiguousarray(inp["ffn_w_gu"], np.float32)
    p["ffn_w_down"] = np.ascontiguousarray(inp["ffn_w_down"], np.float32)
    for k in ("moe_w_router", "moe_w_gu", "moe_w_down", "c_w_in", "c_w_out", "d_w_in", "d_w_o"):
        p[k] = np.ascontiguousarray(inp[k], np.float32)
    for k in ("b_w1", "b_a1", "b_g1", "b_w2", "b_a2", "b_g2", "b_w_r", "b_w_k", "b_w_v", "b_w_o"):
        p[k] = np.ascontiguousarray(inp[k], np.float32)
    bp = np.zeros((128, 8, 11), np.float32)
    bmix = np.asarray(inp["b_mix"], np.float32)
    for j in range(6):
        bp[:, :, j] = colT(bmix[j], 8)
    bp[:, :, 6] = colT(inp["b_w0"], 8)
    bp[:, :, 7] = colT(inp["b_a0"], 8)
    bp[:, :, 8] = colT(inp["b_k_k"], 8)
    bp[:, :, 9] = colT(inp["b_k_a"], 8)
    bp[:, :, 10] = colT(np.asarray(inp["b_r_k"], np.float32).reshape(-1), 8)
    p["b_prm"] = bp
    gg = np.asarray(inp["b_gn_g"], np.float32).reshape(8, 2, 64)
    gb = np.asarray(inp["b_gn_b"], np.float32).reshape(8, 2, 64)
    gt = np.zeros((128, 8, 128), np.float32)
    for hh in range(2):
        gt[hh * 64:(hh + 1) * 64, :, 0:64] = gg[:, hh, :][None, :, :]
        gt[hh * 64:(hh + 1) * 64, :, 64:128] = gb[:, hh, :][None, :, :]
    p["b_gn_tok"] = gt
    cw = np.asarray(inp["c_conv_w"], np.float32)
    prm = np.zeros((128, 10, 8), np.float32)
    for kk in range(4):
        prm[:, :, kk] = colT(cw[kk], 10)
    prm[:, :, 4] = colT(inp["c_conv_b"], 10)
    prm[:, :, 5] = colT(inp["c_b_ga"], 10)
    prm[:, :, 6] = colT(inp["c_b_gx"], 10)
    prm[:, :, 7] = colT(inp["c_lam"], 10)
    p["c_prm"] = prm
    p["c_w_gaT"] = np.ascontiguousarray(np.asarray(inp["c_w_ga"], np.float32).transpose(1, 0, 2))
    p["c_w_gxT"] = np.ascontiguousarray(np.asarray(inp["c_w_gx"], np.float32).transpose(1, 0, 2))
    return p


ALL_STAGES = [("mixA", 0), ("ffn", 0), ("mixB", 1), ("moe", 1), ("mixC", 2), ("ffn", 2), ("mixD", 3), ("moe", 3)]


def run_stages(inp, stages, trace=False):
    nc = build(stages)
    params = prep_params(inp)
    consts = make_consts()
    x = np.asarray(inp["x"], np.float32)
    in_maps = []
    need = needed_params(stages)
    pos = np.asarray(inp["positions"]).astype(np.int32)
    for b in range(N_CORES):
        m = {"xT": np.ascontiguousarray(x[b].T)}
        params["pos"] = np.ascontiguousarray(pos[b].reshape(1, S))
        m.update({k: params[k] for k in need})
        m.update(consts)
        in_maps.append(m)
    res = run_bass_kernel_spmd(nc, in_maps, core_ids=list(range(N_CORES)), trace=trace)
    out = np.stack([np.ascontiguousarray(r["outT"].T) for r in res.results], axis=0)
    return out.astype(np.float32), res


def kernel(**inputs):
    out, _ = run_stages(inputs, ALL_STAGES)
    return out
```
